# Optimizing a Trainium2 kernel written in Bass

```python
import jax, jax.numpy as jnp
from jax import lax
import numpy as np

D_MODEL = 1024
BATCH = 16
SEQ = 4096
DEPTH = 1

MEM_LEN = 256
ROPE_THETA = 10000.0
EPS = 1e-6
Q_BLOCK = 128

DSA_HEADS = 8
DSA_HEAD_DIM = 64
IDX_HEADS = 8
IDX_DIM = 64
TOPK_MAX = 256
MLA_HEADS = 8
MLA_NOPE = 64
MLA_ROPE = 32
MLA_QK = MLA_NOPE + MLA_ROPE
MLA_V = 64
MLA_Q_RANK = 256
MLA_KV_RANK = 128
MEM_HEADS = 4
MEM_HEAD_DIM = 64
N_GROUPS = 4
EXPERTS_PER_GROUP = 4
N_EXPERTS = N_GROUPS * EXPERTS_PER_GROUP
TOPK_IN_GROUP = 2
D_EXPERT = 256

DSA_WIDTH = DSA_HEADS * DSA_HEAD_DIM
MLA_WIDTH = MLA_HEADS * MLA_V
IN_SIZES = (DSA_WIDTH, DSA_HEAD_DIM, DSA_HEAD_DIM, IDX_HEADS * IDX_DIM, IDX_DIM, IDX_HEADS,
            MLA_Q_RANK, MLA_KV_RANK, MLA_ROPE, D_MODEL, D_MODEL)
IN_OFFSETS = tuple(int(v) for v in np.cumsum(IN_SIZES)[:-1])
N_IN = int(sum(IN_SIZES))

kernel_name = 'hybrid_dsa_mla_hmoe_block'


def rms_norm(x, g):
    xf = x.astype(jnp.float32)
    y = xf * lax.rsqrt(jnp.mean(xf * xf, axis=-1, keepdims=True) + EPS)
    return (y * g.astype(jnp.float32)).astype(x.dtype)


def rope(x, pos):
    d = x.shape[-1]
    half = d // 2
    inv_freq = ROPE_THETA ** (-jnp.arange(half, dtype=jnp.float32) / half)
    ang = pos.astype(jnp.float32)[..., None] * inv_freq
    ang = ang.reshape(ang.shape[:2] + (1,) * (x.ndim - 3) + (half,))
    cos, sin = jnp.cos(ang), jnp.sin(ang)
    xf = x.astype(jnp.float32)
    x1, x2 = xf[..., :half], xf[..., half:]
    return jnp.concatenate([x1 * cos - x2 * sin, x2 * cos + x1 * sin], axis=-1).astype(x.dtype)


def to_blocks(a):
    b, s = a.shape[:2]
    return jnp.moveaxis(a.reshape((b, s // Q_BLOCK, Q_BLOCK) + a.shape[2:]), 1, 0)


def from_blocks(a):
    nb, b, q = a.shape[:3]
    return jnp.moveaxis(a, 0, 1).reshape((b, nb * q) + a.shape[3:])


def gather_rows(table, idx):
    return jax.vmap(lambda tb, ib: tb[ib])(table, idx)


def dsa_attention(q, k, v, q_idx, k_idx, w_idx):
    s_len = q.shape[1]
    topk = min(TOPK_MAX, s_len // 4)
    key_pos = jnp.arange(s_len, dtype=jnp.int32)
    idx_scale = IDX_DIM ** -0.5 * IDX_HEADS ** -0.5
    att_scale = DSA_HEAD_DIM ** -0.5

    def block(args):
        qb, qib, wb, start = args
        qpos = start + jnp.arange(Q_BLOCK, dtype=jnp.int32)
        rel = jax.nn.relu(jnp.einsum('bqhd,bkd->bqhk', qib, k_idx).astype(jnp.float32))
        score = jnp.einsum('bqhk,bqh->bqk', rel, wb.astype(jnp.float32)) * idx_scale
        causal = key_pos[None, :] <= qpos[:, None]
        score = jnp.where(causal[None], score, -jnp.inf)
        _, sel = lax.top_k(score, topk)
        valid = sel <= qpos[None, :, None]
        ks = gather_rows(k, sel)
        vs = gather_rows(v, sel)
        logits = jnp.einsum('bqhd,bqkd->bqhk', qb, ks).astype(jnp.float32) * att_scale
        logits = jnp.where(valid[:, :, None, :], logits, -jnp.inf)
        p = jax.nn.softmax(logits, axis=-1).astype(vs.dtype)
        return jnp.einsum('bqhk,bqkd->bqhd', p, vs)

    starts = jnp.arange(s_len // Q_BLOCK, dtype=jnp.int32) * Q_BLOCK
    out = lax.map(block, (to_blocks(q), to_blocks(q_idx), to_blocks(w_idx), starts))
    return from_blocks(out)


def causal_block_attention(q, k, v, scale):
    s_len = q.shape[1]
    key_pos = jnp.arange(s_len, dtype=jnp.int32)

    def block(args):
        qb, start = args
        qpos = start + jnp.arange(Q_BLOCK, dtype=jnp.int32)
        logits = jnp.einsum('bqhd,bkhd->bhqk', qb, k).astype(jnp.float32) * scale
        mask = key_pos[None, :] <= qpos[:, None]
        logits = jnp.where(mask[None, None], logits, -jnp.inf)
        p = jax.nn.softmax(logits, axis=-1).astype(v.dtype)
        return jnp.einsum('bhqk,bkhd->bqhd', p, v)

    starts = jnp.arange(s_len // Q_BLOCK, dtype=jnp.int32) * Q_BLOCK
    return from_blocks(lax.map(block, (to_blocks(q), starts)))


def parallel_mixer(h, pos, w_in, dsa_q_g, dsa_k_g, mla_cq_g, mla_ckv_g, mla_w_uq, mla_w_ukv,
                   mla_q_g, mla_k_g, w_br_dsa, w_br_mla, w_out):
    b, s, _ = h.shape
    proj = h @ w_in
    dq, dk, dv, iq, ik, iw, cq, ckv, kpe, gd, gm = jnp.split(proj, IN_OFFSETS, axis=-1)

    q_a = rope(rms_norm(dq.reshape(b, s, DSA_HEADS, DSA_HEAD_DIM), dsa_q_g), pos)
    k_a = rope(rms_norm(dk, dsa_k_g), pos)
    q_i = rope(iq.reshape(b, s, IDX_HEADS, IDX_DIM), pos)
    k_i = rope(ik, pos)
    out_a = dsa_attention(q_a, k_a, dv, q_i, k_i, iw).reshape(b, s, DSA_WIDTH)

    q_b = (rms_norm(cq, mla_cq_g) @ mla_w_uq).reshape(b, s, MLA_HEADS, MLA_QK)
    kv_b = (rms_norm(ckv, mla_ckv_g) @ mla_w_ukv).reshape(b, s, MLA_HEADS, MLA_NOPE + MLA_V)
    k_nope, v_b = kv_b[..., :MLA_NOPE], kv_b[..., MLA_NOPE:]
    k_pe = jnp.broadcast_to(kpe[:, :, None, :], (b, s, MLA_HEADS, MLA_ROPE))
    k_b = jnp.concatenate([k_nope, k_pe], axis=-1)
    q_b = rms_norm(q_b, mla_q_g)
    k_b = rms_norm(k_b, mla_k_g)
    q_b = jnp.concatenate([q_b[..., :MLA_NOPE], rope(q_b[..., MLA_NOPE:], pos)], axis=-1)
    k_b = jnp.concatenate([k_b[..., :MLA_NOPE], rope(k_b[..., MLA_NOPE:], pos)], axis=-1)
    out_b = causal_block_attention(q_b, k_b, v_b, MLA_QK ** -0.5).reshape(b, s, MLA_WIDTH)

    merged = jax.nn.sigmoid(gd) * (out_a @ w_br_dsa) + jax.nn.sigmoid(gm) * (out_b @ w_br_mla)
    return merged @ w_out


def memory_cross_attention(h, mem, mem_g, w_q, w_kv, q_g, k_g, w_o):
    b, s, _ = h.shape
    m = mem.shape[1]
    q = rms_norm((h @ w_q).reshape(b, s, MEM_HEADS, MEM_HEAD_DIM), q_g)
    kv = (rms_norm(mem, mem_g) @ w_kv).reshape(b, m, 2, MEM_HEADS, MEM_HEAD_DIM)
    k = rms_norm(kv[:, :, 0], k_g)
    v = kv[:, :, 1]
    logits = jnp.einsum('bshd,bmhd->bhsm', q, k).astype(jnp.float32) * MEM_HEAD_DIM ** -0.5
    p = jax.nn.softmax(logits, axis=-1).astype(v.dtype)
    out = jnp.einsum('bhsm,bmhd->bshd', p, v).reshape(b, s, MEM_HEADS * MEM_HEAD_DIM)
    return out @ w_o


def hierarchical_moe(h, w_group, w_expert, expert_bias, w_gate, w_up, w_down):
    b, s, _ = h.shape
    p_group = jax.nn.softmax((h @ w_group).astype(jnp.float32), axis=-1)
    g_sel = jnp.argmax(p_group, axis=-1)
    p_g = jnp.take_along_axis(p_group, g_sel[..., None], axis=-1)
    aff = jax.nn.sigmoid((h @ w_expert).astype(jnp.float32)).reshape(b, s, N_GROUPS, EXPERTS_PER_GROUP)
    aff_grp = jnp.take_along_axis(aff, g_sel[..., None, None], axis=2)[..., 0, :]
    bias_grp = expert_bias.astype(jnp.float32).reshape(N_GROUPS, EXPERTS_PER_GROUP)[g_sel]
    _, local = lax.top_k(aff_grp + bias_grp, TOPK_IN_GROUP)
    a_sel = jnp.take_along_axis(aff_grp, local, axis=-1)
    w_sel = p_g * a_sel / jnp.sum(a_sel, axis=-1, keepdims=True)
    expert_id = g_sel[..., None] * EXPERTS_PER_GROUP + local
    combine = jnp.sum(jax.nn.one_hot(expert_id, N_EXPERTS, dtype=jnp.float32) * w_sel[..., None], axis=-2)
    y = jnp.zeros(h.shape, jnp.float32)
    for e in range(N_EXPERTS):
        act = jax.nn.silu(h @ w_gate[e]) * (h @ w_up[e])
        y = y + combine[..., e:e + 1] * (act @ w_down[e]).astype(jnp.float32)
    return y.astype(h.dtype)


def setup_inputs(seed: int = 0) -> dict:
    key = jax.random.key(seed)
    ks = iter(jax.random.split(key, 40))

    def w(shape, fan_in):
        return jax.random.normal(next(ks), shape, jnp.float32) * fan_in ** -0.5

    def gain(shape):
        return 1.0 + 0.02 * jax.random.normal(next(ks), shape, jnp.float32)

    L = DEPTH
    x = jax.random.normal(next(ks), (BATCH, SEQ, D_MODEL), jnp.float32)
    mem = jax.random.normal(next(ks), (BATCH, MEM_LEN, D_MODEL), jnp.float32)
    offsets = jax.random.randint(next(ks), (BATCH, 1), 0, 128, dtype=jnp.int32)
    positions = offsets + jnp.arange(SEQ, dtype=jnp.int32)[None, :]
    return {
        'x': x,
        'mem': mem,
        'positions': positions,
        'attn_norm_g': gain((L, D_MODEL)),
        'w_in': w((L, D_MODEL, N_IN), D_MODEL),
        'dsa_q_norm_g': gain((L, DSA_HEAD_DIM)),
        'dsa_k_norm_g': gain((L, DSA_HEAD_DIM)),
        'mla_cq_norm_g': gain((L, MLA_Q_RANK)),
        'mla_ckv_norm_g': gain((L, MLA_KV_RANK)),
        'mla_w_uq': w((L, MLA_Q_RANK, MLA_HEADS * MLA_QK), MLA_Q_RANK),
        'mla_w_ukv': w((L, MLA_KV_RANK, MLA_HEADS * (MLA_NOPE + MLA_V)), MLA_KV_RANK),
        'mla_q_norm_g': gain((L, MLA_QK)),
        'mla_k_norm_g': gain((L, MLA_QK)),
        'w_branch_dsa': w((L, DSA_WIDTH, D_MODEL), DSA_WIDTH),
        'w_branch_mla': w((L, MLA_WIDTH, D_MODEL), MLA_WIDTH),
        'w_out': w((L, D_MODEL, D_MODEL), D_MODEL),
        'mem_x_norm_g': gain((L, D_MODEL)),
        'mem_norm_g': gain((L, D_MODEL)),
        'mem_w_q': w((L, D_MODEL, MEM_HEADS * MEM_HEAD_DIM), D_MODEL),
        'mem_w_kv': w((L, D_MODEL, 2 * MEM_HEADS * MEM_HEAD_DIM), D_MODEL),
        'mem_q_norm_g': gain((L, MEM_HEAD_DIM)),
        'mem_k_norm_g': gain((L, MEM_HEAD_DIM)),
        'mem_w_o': w((L, MEM_HEADS * MEM_HEAD_DIM, D_MODEL), MEM_HEADS * MEM_HEAD_DIM),
        'moe_norm_g': gain((L, D_MODEL)),
        'moe_w_group': w((L, D_MODEL, N_GROUPS), D_MODEL),
        'moe_w_expert': w((L, D_MODEL, N_EXPERTS), D_MODEL),
        'moe_expert_bias': 0.01 * jax.random.normal(next(ks), (L, N_EXPERTS), jnp.float32),
        'moe_w_gate': w((L, N_EXPERTS, D_MODEL, D_EXPERT), D_MODEL),
        'moe_w_up': w((L, N_EXPERTS, D_MODEL, D_EXPERT), D_MODEL),
        'moe_w_down': w((L, N_EXPERTS, D_EXPERT, D_MODEL), D_EXPERT),
    }


def reference(x, mem, positions, attn_norm_g, w_in, dsa_q_norm_g, dsa_k_norm_g, mla_cq_norm_g,
              mla_ckv_norm_g, mla_w_uq, mla_w_ukv, mla_q_norm_g, mla_k_norm_g, w_branch_dsa,
              w_branch_mla, w_out, mem_x_norm_g, mem_norm_g, mem_w_q, mem_w_kv, mem_q_norm_g,
              mem_k_norm_g, mem_w_o, moe_norm_g, moe_w_group, moe_w_expert, moe_expert_bias,
              moe_w_gate, moe_w_up, moe_w_down):
    for l in range(DEPTH):
        h = rms_norm(x, attn_norm_g[l])
        x = x + parallel_mixer(h, positions, w_in[l], dsa_q_norm_g[l], dsa_k_norm_g[l],
                               mla_cq_norm_g[l], mla_ckv_norm_g[l], mla_w_uq[l], mla_w_ukv[l],
                               mla_q_norm_g[l], mla_k_norm_g[l], w_branch_dsa[l], w_branch_mla[l],
                               w_out[l])
        h = rms_norm(x, mem_x_norm_g[l])
        x = x + memory_cross_attention(h, mem, mem_norm_g[l], mem_w_q[l], mem_w_kv[l],
                                       mem_q_norm_g[l], mem_k_norm_g[l], mem_w_o[l])
        h = rms_norm(x, moe_norm_g[l])
        x = x + hierarchical_moe(h, moe_w_group[l], moe_w_expert[l], moe_expert_bias[l],
                                 moe_w_gate[l], moe_w_up[l], moe_w_down[l])
    return x
```

```python
import math
import numpy as np
import concourse.bass as bass
import concourse.mybir as mybir
from concourse.bass_utils import run_bass_kernel_spmd

F32 = mybir.dt.float32
BF16 = mybir.dt.bfloat16
I32 = mybir.dt.int32
ALU = mybir.AluOpType
AF = mybir.ActivationFunctionType
AX = mybir.AxisListType

NCORES = 8
D = 1024
S = 4096
NB = 2
NTS = S // 128
NT = NB * NTS
N_IN = 3688
EPS = 1e-6
NEG = -30000.0
EPOCH = 8192
NBIS = 16


class Res:
    __slots__ = ("name", "w", "rd", "sem", "cnt", "pend")

    def __init__(self, name):
        self.name = name
        self.w = None
        self.rd = []
        self.sem = None
        self.cnt = 0
        self.pend = None


class Op:
    __slots__ = ("eng", "idx", "fn", "waits", "signals", "dma", "semres", "val", "rank")

    def __init__(self, eng, idx, fn, dma, semres):
        self.eng = eng
        self.idx = idx
        self.fn = fn
        self.waits = []
        self.signals = False
        self.dma = dma
        self.semres = semres
        self.val = 0
        self.rank = -1


class T:
    def __init__(self, S_, name, shape, dtype, space="sb", kind=None):
        nc = S_.nc
        self.name = name
        if space == "sb":
            self.t = S_.stack.enter_context(nc.sbuf_tensor(name, list(shape), dtype))
        elif space == "ps":
            self.t = S_.stack.enter_context(nc.psum_tensor(name, list(shape), dtype))
        else:
            if kind is None:
                self.t = nc.dram_tensor(name, list(shape), dtype)
            else:
                self.t = nc.dram_tensor(name, list(shape), dtype, kind=kind)
        self.space = space
        self.res = Res(name)
        if space == "dr" and kind != "ExternalInput":
            self.res.pend = {}

    def __getitem__(self, k):
        if self.space == "dr":
            return self.t.ap()[k]
        return self.t[k]

    def ap(self):
        if self.space == "dr":
            return self.t.ap()
        return self.t[:]


ENGS = ("sp", "act", "dve", "pool", "pe")


class Sched:
    def __init__(self, nc, stack):
        self.nc = nc
        self.stack = stack
        self.semstack = stack
        self.ops = {e: [] for e in ENGS}
        self.seen = {e: {} for e in ENGS}
        self.semres_list = []

    @staticmethod
    def _res(x):
        return x.res if hasattr(x, 'res') else x

    def op(self, eng, meth, reads=(), writes=(), *args, dma=False, semres=None, **kw):
        lst = self.ops[eng]
        if dma:
            semres = self._res(semres)
        o = Op(eng, len(lst), (meth, args, kw), dma, semres)
        deps = []
        for r in reads:
            r = self._res(r)
            if r.pend is not None:
                for (sr, v) in r.pend.values():
                    fake = Op("sp", -1, None, True, sr)
                    fake.val = v
                    deps.append((fake, True))
                continue
            if r.w is not None:
                deps.append((r.w, True))
        for w in writes:
            w = self._res(w)
            if w.pend is not None:
                continue
            if w.w is not None and not (dma and w.w.dma):
                deps.append((w.w, False))
            for rd in w.rd:
                deps.append((rd, False))
        seen = self.seen[eng]
        dmax = {}
        for (p, raw) in deps:
            if p.dma:
                k_ = id(p.semres)
                if k_ not in dmax or dmax[k_].val < p.val:
                    dmax[k_] = p
        deps = [(p, raw) for (p, raw) in deps if not p.dma] + [(p, True) for p in dmax.values()]
        emax = {}
        for (p, raw) in deps:
            if not p.dma:
                if p.eng not in emax or emax[p.eng].idx < p.idx:
                    emax[p.eng] = p
        deps = [(p, raw) for (p, raw) in deps if p.dma] + [(p, True) for p in emax.values()]
        for (p, raw) in deps:
            if p is o:
                continue
            if p.dma:
                key = ("d", id(p.semres))
                if seen.get(key, 0) >= p.val:
                    continue
                seen[key] = p.val
                o.waits.append(p)
            else:
                if p.eng == eng:
                    if eng == "pe" and not dma:
                        continue
                key = ("e", p.eng)
                if seen.get(key, -1) >= p.idx:
                    continue
                seen[key] = p.idx
                p.signals = True
                o.waits.append(p)
        if dma:
            if semres.sem is None:
                semres.sem = self.semstack.enter_context(self.nc.semaphore("ds_" + semres.name))
                self.semres_list.append(semres)
            semres.cnt += 16
            o.val = semres.cnt
        for r in reads:
            r = self._res(r)
            if r.pend is None:
                r.rd.append(o)
        for w in writes:
            w = self._res(w)
            if w.pend is not None:
                assert dma
                w.pend[id(semres)] = (semres, o.val)
                continue
            w.w = o
            w.rd = []
        lst.append(o)
        return o

    def push(self):
        from contextlib import ExitStack
        self._saved = getattr(self, "_saved", [])
        self._saved.append(self.stack)
        self.stack = ExitStack()

    def pop(self):
        self.barrier()
        self.stack.close()
        self.stack = self._saved.pop()

    def barrier(self):
        last = {e: (self.ops[e][-1] if self.ops[e] else None) for e in ENGS}
        lastc = {}
        for e in ENGS:
            lc = None
            for o in reversed(self.ops[e]):
                if not o.dma and o.fn is not None:
                    lc = o
                    break
            lastc[e] = lc
        dmas = [(r, r.cnt) for r in self.semres_list]
        for e in ENGS:
            o = Op(e, len(self.ops[e]), None, False, None)
            seen = self.seen[e]
            for e2 in ENGS:
                p = lastc[e2]
                if p is None or e2 == e:
                    continue
                if seen.get(("e", e2), -1) >= p.idx:
                    continue
                seen[("e", e2)] = p.idx
                p.signals = True
                o.waits.append(p)
            for (r, cnt) in dmas:
                key = ("d", id(r))
                if seen.get(key, 0) >= cnt:
                    continue
                seen[key] = cnt
                fake = Op("sp", -1, None, True, r)
                fake.val = cnt
                o.waits.append(fake)
            self.ops[e].append(o)

    def dma(self, eng, out, in_, reads, writes, semres, **kw):
        return self.op(eng, "dma_start", reads, writes, dma=True, semres=semres, out=out, in_=in_, **kw)

    def finalize(self, final_wait_eng="sp"):
        nc = self.nc
        fin = Op(final_wait_eng, len(self.ops[final_wait_eng]), None, False, None)
        finwaits = [(r.sem, r.cnt) for r in self.semres_list]
        engsems = {}
        for e in ENGS:
            rank = 0
            for o in self.ops[e]:
                if o.signals and not o.dma:
                    o.rank = rank
                    rank += 1
            nep = (rank + EPOCH - 1) // EPOCH
            engsems[e] = [self.semstack.enter_context(nc.semaphore("es_%s_%d" % (e, i))) for i in range(nep)]
        self.engsems = engsems
        nsem = sum(len(v) for v in engsems.values()) + len(self.semres_list)
        self.nsem = nsem

        def replay(ename, e):
            for o in self.ops[ename]:
                for p in o.waits:
                    if p.dma:
                        e.wait_ge(p.semres.sem, p.val)
                    else:
                        e.wait_ge(engsems[p.eng][p.rank // EPOCH], p.rank % EPOCH + 1)
                if o.fn is None:
                    continue
                meth, args, kw = o.fn
                inst = getattr(e, meth)(*args, **kw)
                if o.dma:
                    inst.then_inc(o.semres.sem, 16)
                elif o.signals:
                    inst.then_inc(engsems[ename][o.rank // EPOCH], 1)
            if ename == final_wait_eng:
                for (sem, cnt) in finwaits:
                    e.wait_ge(sem, cnt)

        with nc.Block() as block:
            @block.sync
            def _(e):
                replay("sp", e)

            @block.scalar
            def _(e):
                replay("act", e)

            @block.vector
            def _(e):
                replay("dve", e)

            @block.gpsimd
            def _(e):
                replay("pool", e)

            @block.tensor
            def _(e):
                replay("pe", e)


def build(phases=(1, 2, 3, 4), debug=(), ntl=NT):
    from contextlib import ExitStack
    nc = bass.Bass("TRN2", target_bir_lowering=False)
    stack = ExitStack()
    S_ = Sched(nc, stack)
    op = S_.op

    def dram_in(name, shape, dtype=F32):
        return T(S_, name, shape, dtype, "dr", kind="ExternalInput")

    x_d = dram_in("x", [NB * S, D])
    mem_d = dram_in("mem", [NB * 256, D])
    pos_d = dram_in("positions", [NT, 128], I32)
    attn_g_d = dram_in("attn_norm_g", [1, D])
    w_in_d = dram_in("w_in", [D, N_IN])
    dsa_qg_d = dram_in("dsa_q_norm_g", [1, 64])
    dsa_kg_d = dram_in("dsa_k_norm_g", [1, 64])
    cq_g_d = dram_in("mla_cq_norm_g", [1, 256])
    ckv_g_d = dram_in("mla_ckv_norm_g", [1, 128])
    w_uq_d = dram_in("mla_w_uq", [256, 768])
    w_ukv_d = dram_in("mla_w_ukv", [128, 1024])
    mq_g_d = dram_in("mla_q_norm_g", [1, 96])
    mk_g_d = dram_in("mla_k_norm_g", [1, 96])
    w_bra_d = dram_in("w_branch_dsa", [512, D])
    w_brb_d = dram_in("w_branch_mla", [512, D])
    w_out_d = dram_in("w_out", [D, D])
    memx_g_d = dram_in("mem_x_norm_g", [1, D])
    mem_g_d = dram_in("mem_norm_g", [1, D])
    mem_wq_d = dram_in("mem_w_q", [D, 256])
    mem_wkv_d = dram_in("mem_w_kv", [D, 512])
    memq_g_d = dram_in("mem_q_norm_g", [1, 64])
    memk_g_d = dram_in("mem_k_norm_g", [1, 64])
    mem_wo_d = dram_in("mem_w_o", [256, D])
    moe_g_d = dram_in("moe_norm_g", [1, D])
    moe_wg_d = dram_in("moe_w_group", [D, 4])
    moe_we_d = dram_in("moe_w_expert", [D, 16])
    moe_b_d = dram_in("moe_expert_bias", [1, 16])
    moe_gate_d = dram_in("moe_w_gate", [16, D, 256])
    moe_up_d = dram_in("moe_w_up", [16, D, 256])
    moe_down_d = dram_in("moe_w_down", [16, 256, D])
    out_d = T(S_, "out", [NB * S, D], F32, "dr", kind="ExternalOutput")

    dbg_kind = lambda n: ("ExternalOutput" if (n in debug or n.startswith("dbg_")) else None)

    def scratch(name, shape, dtype):
        return T(S_, name, shape, dtype, "dr", kind=dbg_kind(name))

    qaT_d = scratch("qaT", [NT, 8, 64, 128], BF16)
    qiT_d = scratch("qiT", [NT, 8, 64, 128], BF16)
    kaT_d = scratch("kaT", [NB, 64, S], BF16)
    kiT_d = scratch("kiT", [NB, 64, S], BF16)
    va_d = scratch("va", [NB * S, 65], BF16)
    wi_d = scratch("wi", [NT, 128, 8], F32)
    qbT_d = scratch("qbT", [NB, 96, 8, S], BF16)
    kbT_d = scratch("kbT", [NB, 96, 8, S], BF16)
    vb_d = scratch("vb", [NB * S, 8 * 65], BF16)
    sg_d = scratch("sg", [NT, 128, 2048], BF16)
    oaT_d = scratch("oaT", [NT, 64, 8, 128], BF16)
    obT_d = scratch("obT", [NT, 64, 8, 128], BF16)
    x2_d = scratch("x2", [NB * S, D], F32)
    wgu_d = scratch("wgu", [16, 128, 8, 512], BF16)
    wgu_sem = Res("wgu_sem")
    if "dbg_negm" in debug:
        dbg_negm = scratch("dbg_negm", [ntl, 128, S], BF16)
        dbg_score = scratch("dbg_score", [ntl, 128, S], F32)

    def sb(name, shape, dtype=F32):
        return T(S_, name, shape, dtype, "sb")

    def ps(name, shape, dtype=F32):
        return T(S_, name, shape, dtype, "ps")

    io_i = sb("io_i", [128, 128], I32)
    io_f = sb("io_f", [128, 128])
    ident_f = sb("ident_f", [128, 128])
    ident_bf = sb("ident_bf", [128, 128], BF16)
    neghalf = sb("neghalf", [128, 16])
    ones_f = sb("ones_f", [128, 64])
    junk_act = sb("junk_act", [128, 1024], BF16)

    op("pool", "iota", [], [io_i], io_i[:, :], [[1, 128]], base=0, channel_multiplier=-1)
    op("dve", "tensor_copy", [io_i], [io_f], io_f[:, :], io_i[:, :])
    op("dve", "tensor_single_scalar", [io_f], [ident_f], ident_f[:, :], io_f[:, :], 0.0, ALU.is_equal)
    op("dve", "tensor_copy", [ident_f], [ident_bf], ident_bf[:, :], ident_f[:, :])
    op("dve", "memset", [], [neghalf], neghalf[:, :], -0.5)
    op("dve", "memset", [], [ones_f], ones_f[:, :], 1.0)

    def bcast_load(name, dt_, n):
        t = sb(name, [128, n])
        S_.dma("sp", t[:, :], dt_[0:1, :].broadcast_to([128, n]), [dt_], [t], t)
        return t

    def rstd_from_ss(ss, out, n, d, tmp):
        op("dve", "tensor_scalar", [ss], [tmp], tmp[:, 0:n], ss[:, 0:n], 1.0 / d, EPS, ALU.mult, ALU.add)
        op("pool", "tensor_tensor", [tmp, neghalf], [out], out[:, 0:n], tmp[:, 0:n], neghalf[:, 0:n], ALU.pow)

    def rms_heads(src3, src_res, dst3, dst_res, H, d, gbc, tmps):
        sq, ssq, r1, rstd, xn = tmps
        sq3 = sq[:, 0:H * d].rearrange("p (h d) -> p h d", h=H)
        xn3 = xn[:, 0:H * d].rearrange("p (h d) -> p h d", h=H)
        op("dve", "tensor_tensor", [src_res], [sq], sq3, src3, src3, ALU.mult)
        op("dve", "tensor_reduce", [sq], [ssq], ssq[:, 0:H], sq3, AX.X, ALU.add)
        rstd_from_ss(ssq, rstd, H, d, r1)
        op("dve", "tensor_tensor", [src_res, rstd], [xn], xn3, src3, rstd[:, 0:H].unsqueeze(2).broadcast_to([128, H, d]), ALU.mult)
        op("dve", "tensor_tensor", [xn, gbc], [dst_res], dst3, xn3, gbc[:, 0:d].unsqueeze(1).broadcast_to([128, H, d]), ALU.mult)

    def rope(src3, src_res, dst3, dst_res, H, d, cs, sn, trig_res, tmps, eng="pool"):
        t1, t2 = tmps
        hd = d // 2
        x1 = src3[:, :, 0:hd]
        x2 = src3[:, :, hd:d]
        csb = cs.unsqueeze(1).broadcast_to([128, H, hd])
        snb = sn.unsqueeze(1).broadcast_to([128, H, hd])
        a = t1[:, 0:H * hd].rearrange("p (h d) -> p h d", h=H)
        b = t2[:, 0:H * hd].rearrange("p (h d) -> p h d", h=H)
        op(eng, "tensor_tensor", [src_res, trig_res], [t1], a, x1, csb, ALU.mult)
        op(eng, "tensor_tensor", [src_res, trig_res], [t2], b, x2, snb, ALU.mult)
        op(eng, "tensor_tensor", [t1, t2], [dst_res], dst3[:, :, 0:hd], a, b, ALU.subtract)
        op(eng, "tensor_tensor", [src_res, trig_res], [t1], a, x2, csb, ALU.mult)
        op(eng, "tensor_tensor", [src_res, trig_res], [t2], b, x1, snb, ALU.mult)
        op(eng, "tensor_tensor", [t1, t2], [dst_res], dst3[:, :, hd:d], a, b, ALU.add)

    class View:
        def __init__(self, base_ap, res):
            self.base = base_ap
            self.res = res

        def __getitem__(self, k):
            return self.base[k]

    P2 = [ps("pz%d" % i, [128, 1024]) for i in range(4)]
    pbank = [View(P2[i // 2][:, (i % 2) * 512:(i % 2) * 512 + 512], Res("pb%d" % i)) for i in range(8)]

    TW = 48
    if 1 in phases:
        S_.push()
        sinT = sb("sinT", [128, NT * TW])
        cosT = sb("cosT", [128, NT * TW])
        S_.push()
        pos_i = sb("pos_i", [NT, 128], I32)
        pos_f = sb("pos_f", [NT, 128])
        posT = sb("posT", [128, NT])
        fr_i = sb("fr_i", [128, TW], I32)
        fr_f = sb("fr_f", [128, TW])
        freq = sb("freq", [128, TW])
        xs = sb("trig_xs", [128, NT * TW])
        xk_i = sb("trig_ki", [128, NT * TW], I32)
        xk_f = sb("trig_kf", [128, NT * TW])
        xa = sb("trig_a", [128, NT * TW])
        S_.dma("sp", pos_i[:, :], pos_d[:, :], [pos_d], [pos_i], pos_i)
        op("dve", "tensor_copy", [pos_i], [pos_f], pos_f[:, :], pos_i[:, :])
        op("pe", "transpose", [pos_f, ident_f], [pbank[0]], pbank[0][:, 0:NT], pos_f[:, :], ident_f[0:NT, 0:NT])
        op("dve", "tensor_copy", [pbank[0]], [posT], posT[:, :], pbank[0][:, 0:NT])
        op("pool", "iota", [], [fr_i], fr_i[:, :], [[1, TW]], base=0, channel_multiplier=0)
        op("dve", "tensor_copy", [fr_i], [fr_f], fr_f[:, :], fr_i[:, :])
        lt = math.log(10000.0)
        op("act", "activation", [fr_f], [freq], freq[:, 0:32], fr_f[:, 0:32], AF.Exp, scale=-lt / 32.0)
        op("act", "activation", [fr_f], [freq], freq[:, 32:48], fr_f[:, 32:48], AF.Exp, scale=-lt / 16.0, bias=2.0 * lt)
        xs3 = xs[:, :].rearrange("p (t w) -> p t w", w=TW)
        op("dve", "tensor_tensor", [posT, freq], [xs], xs3, posT[:, :].unsqueeze(2).broadcast_to([128, NT, TW]),
                                            freq[:, :].unsqueeze(1).broadcast_to([128, NT, TW]), ALU.mult)
        op("dve", "tensor_scalar", [xs], [xs], xs[:, :], xs[:, :], 1.0 / (2 * math.pi), None, ALU.mult)

        def trig(dst, shift):
            op("dve", "tensor_scalar", [xs], [xa], xa[:, :], xs[:, :], shift, None, ALU.add)
            op("dve", "tensor_copy", [xa], [xk_i], xk_i[:, :], xa[:, :])
            op("dve", "tensor_copy", [xk_i], [xk_f], xk_f[:, :], xk_i[:, :])
            op("dve", "tensor_tensor", [xa, xk_f], [xa], xa[:, :], xa[:, :], xk_f[:, :], ALU.subtract)
            op("dve", "tensor_single_scalar", [xa], [xk_f], xk_f[:, :], xa[:, :], 0.5, ALU.is_gt)
            op("dve", "tensor_tensor", [xa, xk_f], [xa], xa[:, :], xa[:, :], xk_f[:, :], ALU.subtract)
            op("dve", "tensor_single_scalar", [xa], [xk_f], xk_f[:, :], xa[:, :], -0.5, ALU.is_lt)
            op("dve", "tensor_tensor", [xa, xk_f], [xa], xa[:, :], xa[:, :], xk_f[:, :], ALU.add)
            op("act", "activation", [xa], [dst], dst[:, :], xa[:, :], AF.Sin, scale=2 * math.pi)

        trig(sinT, 0.0)
        trig(cosT, 0.25)
        S_.pop()

        sin3 = sinT[:, :].rearrange("p (t w) -> p t w", w=TW)
        cos3 = cosT[:, :].rearrange("p (t w) -> p t w", w=TW)

    if 1 in phases:
        g_attn = bcast_load("g_attn", attn_g_d, D)
        g_dq = bcast_load("g_dq", dsa_qg_d, 64)
        g_dk = bcast_load("g_dk", dsa_kg_d, 64)
        g_cq = bcast_load("g_cq", cq_g_d, 256)
        g_ckv = bcast_load("g_ckv", ckv_g_d, 128)
        g_mq = bcast_load("g_mq", mq_g_d, 96)
        g_mk = bcast_load("g_mk", mk_g_d, 96)
        win = sb("win", [128, 8 * N_IN], BF16)
        win3 = win[:, :].rearrange("p (k n) -> p k n", k=8)
        for kc in range(8):
            S_.dma("pool", win3[:, kc, :], w_in_d[kc * 128:(kc + 1) * 128, :], [w_in_d], [win], win)
        wuq = sb("wuq", [128, 2 * 768], BF16)
        wuq3 = wuq[:, :].rearrange("p (k n) -> p k n", k=2)
        for kc in range(2):
            S_.dma("pool", wuq3[:, kc, :], w_uq_d[kc * 128:(kc + 1) * 128, :], [w_uq_d], [wuq], wuq)
        wukv = sb("wukv", [128, 1024], BF16)
        S_.dma("pool", wukv[:, :], w_ukv_d[:, :], [w_ukv_d], [wukv], wukv)
        if 4 in phases:
            for e in range(16):
                S_.dma("pool", wgu_d[e][:, :, 0:256], moe_gate_d[e].rearrange("(k p) f -> p k f", p=128), [moe_gate_d], [wgu_d], wgu_sem)
                S_.dma("pool", wgu_d[e][:, :, 256:512], moe_up_d[e].rearrange("(k p) f -> p k f", p=128), [moe_up_d], [wgu_d], wgu_sem)


        xin = [sb("xin%d" % i, [128, D]) for i in range(2)]
        ss = sb("ss", [128, 16])
        r1 = sb("r1", [128, 16])
        rstd = sb("rstd", [128, 16])
        h_bf = sb("h_bf", [128, D], BF16)
        hT = sb("hT", [128, D], BF16)
        proj2 = [sb("proj%d" % i, [128, N_IN]) for i in range(2)]
        t_sq = sb("t_sq", [128, 1024])
        t_xn = sb("t_xn", [128, 1024])
        t_ssq = sb("t_ssq", [128, 16])
        ssqA = sb("ssqA", [128, 16])
        eps_t = sb("eps_t", [128, 1])
        op("dve", "memset", [], [eps_t], eps_t[:, :], EPS)
        r1A = sb("r1A", [128, 16])
        rstdA = sb("rstdA", [128, 16])
        ssqB = sb("ssqB", [128, 16])
        r1B = sb("r1B", [128, 16])
        rstdB = sb("rstdB", [128, 16])
        invdA = sb("invdA", [128, 16])
        invdB = sb("invdB", [128, 16])
        op("dve", "memset", [], [invdA], invdA[:, :], 1.0 / 64)
        op("dve", "memset", [], [invdA], invdA[:, 9:10], 1.0 / 256)
        op("dve", "memset", [], [invdA], invdA[:, 10:11], 1.0 / 128)
        op("dve", "memset", [], [invdB], invdB[:, :], 1.0 / 96)
        t_r1 = sb("t_r1", [128, 16])
        t_rstd = sb("t_rstd", [128, 16])
        tmps = (t_sq, t_ssq, t_r1, t_rstd, t_xn)
        t_a = sb("t_a", [128, 512])
        t_b = sb("t_b", [128, 512])
        t_c = sb("t_c", [128, 256])
        t_d = sb("t_d", [128, 256])
        qan = sb("qan", [128, 512])
        qa_sb = sb("qa_sb", [128, 512], BF16)
        qi_sb = sb("qi_sb", [128, 512], BF16)
        kk_n = sb("kk_n", [128, 64])
        kk_sb = sb("kk_sb", [128, 128], BF16)
        cqn = sb("cqn", [128, 256], BF16)
        cqT = sb("cqT", [128, 256], BF16)
        ckvn = sb("ckvn", [128, 128], BF16)
        ckvT = sb("ckvT", [128, 128], BF16)
        qb_f = sb("qb_f", [128, 768])
        qb_n = sb("qb_n", [128, 768])
        qb_sb = sb("qb_sb", [128, 768], BF16)
        kv_f = sb("kv_f", [128, 1024])
        kb_f = sb("kb_f", [128, 768])
        kb_n = sb("kb_n", [128, 768])
        kb_sb = sb("kb_sb", [128, 768], BF16)
        qaT_s = [sb("qaT_s%d" % i, [128, 512], BF16) for i in range(2)]
        qiT_s = [sb("qiT_s%d" % i, [128, 512], BF16) for i in range(2)]
        kkT_s = [sb("kkT_s%d" % i, [128, 128], BF16) for i in range(2)]
        va_s = [sb("va_s%d" % i, [128, 65], BF16) for i in range(2)]
        wi_s = [sb("wi_s%d" % i, [128, 8]) for i in range(2)]
        qbT_s = [sb("qbT_s%d" % i, [128, 1024], BF16) for i in range(2)]
        kbT_s = [sb("kbT_s%d" % i, [128, 1024], BF16) for i in range(2)]
        vb_s = [sb("vb_s%d" % i, [128, 8 * 65], BF16) for i in range(2)]
        sg_s = [sb("sg_s%d" % i, [128, 2048], BF16) for i in range(2)]
        for i in range(2):
            op("dve", "memset", [], [va_s[i]], va_s[i][:, 64:65], 1.0)
            op("dve", "memset", [], [vb_s[i]], vb_s[i][:, :], 1.0)

        def pbf(i):
            return pbank[i][:, :].bitcast(BF16)

        OFF = [0, 512, 576, 640, 1152, 1216, 1224, 1480, 1608, 1640, 2664, 3688]
        evac_flip = [0]

        def evac(dst_ap, dst_res, src_ap, src_res):
            if evac_flip[0] % 2 == 0:
                op("act", "copy", [src_res], [dst_res], dst_ap, src_ap)
            else:
                op("dve", "tensor_copy", [src_res], [dst_res], dst_ap, src_ap)
            evac_flip[0] += 1

        S_.dma("sp", xin[0][:, :], x_d[0:128, :], [x_d], [xin[0]], xin[0])
        def stageA(ti):
            proj = proj2[ti % 2]
            b, t = divmod(ti, NTS)
            sl = ti % 2
            xt = xin[sl]
            if ti + 1 < ntl:
                S_.dma("sp", xin[1 - sl][:, :], x_d[(ti + 1) * 128:(ti + 2) * 128, :], [x_d], [xin[1 - sl]], xin[1 - sl])
            op("act", "activation", [xt], [ss], junk_act[:, :], xt[:, :], AF.Square, accum_out=ss[:, 0:1])
            op("act", "activation", [ss, eps_t], [r1], r1[:, 0:1], ss[:, 0:1], AF.Sqrt, scale=1.0 / D, bias=eps_t[:, 0:1])
            op("dve", "reciprocal", [r1], [rstd], rstd[:, 0:1], r1[:, 0:1])
            op("dve", "scalar_tensor_tensor", [xt, rstd, g_attn], [h_bf], h_bf[:, :], xt[:, :], rstd[:, 0:1], g_attn[:, :], ALU.mult, ALU.mult)
            yield
            tpb = 4 + (ti % 2)
            for kc in range(8):
                op("pe", "transpose", [h_bf, ident_bf], [pbank[tpb]], pbf(tpb)[:, kc * 128:(kc + 1) * 128], h_bf[:, kc * 128:(kc + 1) * 128], ident_bf[:, :])
            op("act", "copy", [pbank[tpb]], [hT], hT[:, :], pbf(tpb)[:, :])
            yield
            for nchunk in range(8):
                c0 = nchunk * 512
                w = min(512, N_IN - c0)
                pb = pbank[nchunk % 2]
                for kc in range(8):
                    op("pe", "matmul", [hT, win], [pb], pb[:, 0:w], hT[:, kc * 128:(kc + 1) * 128], win3[:, kc, c0:c0 + w],
                                                                          start=(kc == 0), stop=(kc == 7))
                op("act", "copy", [pb], [proj], proj[:, c0:c0 + w], pb[:, 0:w])
                yield

        def stageB(ti):
            proj = proj2[ti % 2]
            b, t = divmod(ti, NTS)
            sl = ti % 2
            cs64 = cos3[:, ti, 0:32]
            sn64 = sin3[:, ti, 0:32]
            cs32 = cos3[:, ti, 32:48]
            sn32 = sin3[:, ti, 32:48]
            trig_res = sinT
            dq3 = proj[:, 0:512].rearrange("p (h d) -> p h d", h=8)
            dk3 = proj[:, 512:576].rearrange("p (h d) -> p h d", h=1)
            cq3 = proj[:, 1224:1480].rearrange("p (h d) -> p h d", h=1)
            ckv3 = proj[:, 1480:1608].rearrange("p (h d) -> p h d", h=1)
            qan3 = qan[:, :].rearrange("p (h d) -> p h d", h=8)
            kkn3 = kk_n[:, :].rearrange("p (h d) -> p h d", h=1)

            def ss_part(src3, src_res, H, d, ssq, col):
                sq3 = t_sq[:, 0:H * d].rearrange("p (h d) -> p h d", h=H)
                op("dve", "tensor_tensor", [src_res], [t_sq], sq3, src3, src3, ALU.mult)
                op("dve", "tensor_reduce", [t_sq], [ssq], ssq[:, col:col + H], sq3, AX.X, ALU.add)

            def rstd_all(ssq, invd, r1_, rstd_, n):
                op("dve", "tensor_tensor", [ssq, invd], [r1_], r1_[:, 0:n], ssq[:, 0:n], invd[:, 0:n], ALU.mult)
                op("act", "activation", [r1_, eps_t], [r1_], r1_[:, 0:n], r1_[:, 0:n], AF.Sqrt, bias=eps_t[:, 0:1])
                op("dve", "reciprocal", [r1_], [rstd_], rstd_[:, 0:n], r1_[:, 0:n])

            def apply_part(src3, src_res, dst3, dst_res, H, d, rstd_, col, gbc):
                xn3 = t_xn[:, 0:H * d].rearrange("p (h d) -> p h d", h=H)
                op("dve", "tensor_tensor", [src_res, rstd_], [t_xn], xn3, src3, rstd_[:, col:col + H].unsqueeze(2).broadcast_to([128, H, d]), ALU.mult)
                op("dve", "tensor_tensor", [t_xn, gbc], [dst_res], dst3, xn3, gbc[:, 0:d].unsqueeze(1).broadcast_to([128, H, d]), ALU.mult)

            ss_part(cq3, proj, 1, 256, ssqA, 9)
            ss_part(ckv3, proj, 1, 128, ssqA, 10)
            ss_part(dq3, proj, 8, 64, ssqA, 0)
            ss_part(dk3, proj, 1, 64, ssqA, 8)
            rstd_all(ssqA, invdA, r1A, rstdA, 11)
            yield
            rope(proj[:, 640:1152].rearrange("p (h d) -> p h d", h=8), proj, qi_sb[:, :].rearrange("p (h d) -> p h d", h=8), qi_sb,
                 8, 64, cs64, sn64, trig_res, (t_a, t_b))
            rope(proj[:, 1152:1216].rearrange("p (h d) -> p h d", h=1), proj, kk_sb[:, 64:128].rearrange("p (h d) -> p h d", h=1), kk_sb,
                 1, 64, cs64, sn64, trig_res, (t_a, t_b))
            op("pool", "tensor_copy", [proj], [va_s[sl]], va_s[sl][:, 0:64], proj[:, 576:640])
            op("pool", "tensor_copy", [proj], [wi_s[sl]], wi_s[sl][:, :], proj[:, 1216:1224])
            yield
            apply_part(cq3, proj, cqn[:, :].rearrange("p (h d) -> p h d", h=1), cqn, 1, 256, rstdA, 9, g_cq)
            apply_part(ckv3, proj, ckvn[:, :].rearrange("p (h d) -> p h d", h=1), ckvn, 1, 128, rstdA, 10, g_ckv)
            yield
            tq = 6
            for kc in range(2):
                op("pe", "transpose", [cqn, ident_bf], [pbank[tq]], pbf(tq)[:, kc * 128:(kc + 1) * 128], cqn[:, kc * 128:(kc + 1) * 128], ident_bf[:, :])
            op("pe", "transpose", [ckvn, ident_bf], [pbank[tq]], pbf(tq)[:, 256:384], ckvn[:, :], ident_bf[:, :])
            op("act", "copy", [pbank[tq]], [cqT, ckvT], cqT[:, :], pbf(tq)[:, 0:256])
            op("act", "copy", [pbank[tq]], [ckvT], ckvT[:, :], pbf(tq)[:, 256:384])
            yield
            for (c0, w, pbi) in ((0, 512, 2), (512, 256, 3)):
                pb = pbank[pbi]
                for kc in range(2):
                    op("pe", "matmul", [cqT, wuq], [pb], pb[:, 0:w], cqT[:, kc * 128:(kc + 1) * 128], wuq3[:, kc, c0:c0 + w],
                       start=(kc == 0), stop=(kc == 1))
                op("act", "copy", [pb], [qb_f], qb_f[:, c0:c0 + w], pb[:, 0:w])
            for (c0, pbi) in ((0, 2), (512, 3)):
                pb = pbank[pbi]
                op("pe", "matmul", [ckvT, wukv], [pb], pb[:, 0:512], ckvT[:, :], wukv[:, c0:c0 + 512], start=True, stop=True)
                op("act", "copy", [pb], [kv_f], kv_f[:, c0:c0 + 512], pb[:, 0:512])
            apply_part(dq3, proj, qan3, qan, 8, 64, rstdA, 0, g_dq)
            yield
            apply_part(dk3, proj, kkn3, kk_n, 1, 64, rstdA, 8, g_dk)
            yield
            rope(qan3, qan, qa_sb[:, :].rearrange("p (h d) -> p h d", h=8), qa_sb, 8, 64, cs64, sn64, trig_res, (t_c, t_d), eng="dve")
            rope(kkn3, kk_n, kk_sb[:, 0:64].rearrange("p (h d) -> p h d", h=1), kk_sb, 1, 64, cs64, sn64, trig_res, (t_c, t_d), eng="dve")
            yield
            qbf3 = qb_f[:, :].rearrange("p (h d) -> p h d", h=8)
            qbn3 = qb_n[:, :].rearrange("p (h d) -> p h d", h=8)
            qbs3 = qb_sb[:, :].rearrange("p (h d) -> p h d", h=8)
            kv3 = kv_f[:, :].rearrange("p (h d) -> p h d", h=8)
            kbf3 = kb_f[:, :].rearrange("p (h d) -> p h d", h=8)
            kbn3 = kb_n[:, :].rearrange("p (h d) -> p h d", h=8)
            kbs3 = kb_sb[:, :].rearrange("p (h d) -> p h d", h=8)
            vbs3 = vb_s[sl][:, :].rearrange("p (h d) -> p h d", h=8)
            op("act", "copy", [kv_f], [kb_f], kbf3[:, :, 0:64], kv3[:, :, 0:64])
            op("pool", "tensor_copy", [proj], [kb_f], kbf3[:, :, 64:96], proj[:, 1608:1640].unsqueeze(1).broadcast_to([128, 8, 32]))
            op("act", "copy", [kv_f], [vb_s[sl]], vbs3[:, :, 0:64], kv3[:, :, 64:128])
            ss_part(qbf3, qb_f, 8, 96, ssqB, 0)
            ss_part(kbf3, kb_f, 8, 96, ssqB, 8)
            rstd_all(ssqB, invdB, r1B, rstdB, 16)
            yield
            apply_part(qbf3, qb_f, qbn3, qb_n, 8, 96, rstdB, 0, g_mq)
            apply_part(kbf3, kb_f, kbn3, kb_n, 8, 96, rstdB, 8, g_mk)
            yield
            op("act", "copy", [qb_n], [qb_sb], qbs3[:, :, 0:64], qbn3[:, :, 0:64])
            rope(qbn3[:, :, 64:96], qb_n, qbs3[:, :, 64:96], qb_sb, 8, 32, cs32, sn32, trig_res, (t_a, t_b))
            op("act", "copy", [kb_n], [kb_sb], kbs3[:, :, 0:64], kbn3[:, :, 0:64])
            rope(kbn3[:, :, 64:96], kb_n, kbs3[:, :, 64:96], kb_sb, 8, 32, cs32, sn32, trig_res, (t_a, t_b))
            yield
            op("act", "activation", [proj], [sg_s[sl]], sg_s[sl][:, :], proj[:, 1640:3688], AF.Sigmoid)
            tr = 7
            for j in range(4):
                op("pe", "transpose", [qa_sb, ident_bf], [pbank[tr]], pbf(tr)[:, j * 128:(j + 1) * 128], qa_sb[:, j * 128:(j + 1) * 128], ident_bf[:, :])
            for j in range(4):
                op("pe", "transpose", [qi_sb, ident_bf], [pbank[tr]], pbf(tr)[:, 512 + j * 128:512 + (j + 1) * 128], qi_sb[:, j * 128:(j + 1) * 128], ident_bf[:, :])
            op("act", "copy", [pbank[tr]], [qaT_s[sl]], qaT_s[sl][:, :], pbf(tr)[:, 0:512])
            op("dve", "tensor_copy", [pbank[tr]], [qiT_s[sl]], qiT_s[sl][:, :], pbf(tr)[:, 512:1024])
            yield
            op("pe", "transpose", [kk_sb, ident_bf], [pbank[tq]], pbf(tq)[:, 384:512], kk_sb[:, :], ident_bf[:, :])
            op("act", "copy", [pbank[tq]], [kkT_s[sl]], kkT_s[sl][:, :], pbf(tq)[:, 384:512])
            for h in range(8):
                op("pe", "transpose", [qb_sb, ident_bf], [pbank[tr]], pbf(tr)[0:96, h * 128:(h + 1) * 128], qb_sb[:, h * 96:(h + 1) * 96], ident_bf[:, :])
            op("act", "copy", [pbank[tr]], [qbT_s[sl]], qbT_s[sl][0:96, :], pbf(tr)[0:96, :])
            yield
            for h in range(8):
                op("pe", "transpose", [kb_sb, ident_bf], [pbank[tr]], pbf(tr)[0:96, h * 128:(h + 1) * 128], kb_sb[:, h * 96:(h + 1) * 96], ident_bf[:, :])
            op("dve", "tensor_copy", [pbank[tr]], [kbT_s[sl]], kbT_s[sl][0:96, :], pbf(tr)[0:96, :])
            tok0 = ti * 128
            S_.dma("sp", qaT_d[ti].rearrange("(j hh) d q -> (hh d) j q", hh=2), qaT_s[sl][:, :].rearrange("p (j q) -> p j q", j=4),
                   [qaT_s[sl]], [qaT_d], qaT_s[sl])
            S_.dma("sp", qiT_d[ti].rearrange("(j hh) d q -> (hh d) j q", hh=2), qiT_s[sl][:, :].rearrange("p (j q) -> p j q", j=4),
                   [qiT_s[sl]], [qiT_d], qiT_s[sl])
            S_.dma("sp", kaT_d[b, :, t * 128:(t + 1) * 128], kkT_s[sl][0:64, :], [kkT_s[sl]], [kaT_d], kkT_s[sl])
            S_.dma("sp", kiT_d[b, :, t * 128:(t + 1) * 128], kkT_s[sl][64:128, :], [kkT_s[sl]], [kiT_d], kkT_s[sl])
            S_.dma("sp", va_d[tok0:tok0 + 128, :], va_s[sl][:, :], [va_s[sl]], [va_d], va_s[sl])
            S_.dma("sp", wi_d[ti], wi_s[sl][:, :], [wi_s[sl]], [wi_d], wi_s[sl])
            S_.dma("sp", qbT_d[b, :, :, t * 128:(t + 1) * 128], qbT_s[sl][0:96, :].rearrange("p (h q) -> p h q", h=8),
                   [qbT_s[sl]], [qbT_d], qbT_s[sl])
            S_.dma("sp", kbT_d[b, :, :, t * 128:(t + 1) * 128], kbT_s[sl][0:96, :].rearrange("p (h q) -> p h q", h=8),
                   [kbT_s[sl]], [kbT_d], kbT_s[sl])
            S_.dma("sp", vb_d[tok0:tok0 + 128, :], vb_s[sl][:, :], [vb_s[sl]], [vb_d], vb_s[sl])
            S_.dma("sp", sg_d[ti], sg_s[sl][:, :], [sg_s[sl]], [sg_d], sg_s[sl])


        def interleave1(gens):
            alive = list(gens)
            while alive:
                for g_ in list(alive):
                    try:
                        next(g_)
                    except StopIteration:
                        alive.remove(g_)

        interleave1([stageA(0)])
        for ti in range(ntl):
            gs_ = [stageB(ti)]
            if ti + 1 < ntl:
                gs_.insert(0, stageA(ti + 1))
            interleave1(gs_)
        S_.pop()

    if 2 in phases:
        phase2(S_, locals())
    if 3 in phases:
        phase3(S_, locals())
    if 4 in phases:
        phase4(S_, locals())
    S_.finalize()
    return nc, stack


class Env:
    def __init__(self, d):
        self.__dict__.update(d)


def phase2(S_, envd):
    E = Env(envd)
    op = S_.op
    sb, pbank, P2 = E.sb, E.pbank, E.P2
    ident_f, ident_bf, io_f, ones_f = E.ident_f, E.ident_bf, E.io_f, E.ones_f
    ntl = E.ntl
    S_.push()
    ident8 = sb("ident8", [128, 1024], BF16)
    op("dve", "tensor_copy", [ident_bf], [ident8], ident8[:, :].rearrange("p (r q) -> p r q", r=8),
       ident_bf[:, :].unsqueeze(1).broadcast_to([128, 8, 128]))
    caus_f = sb("caus_f", [128, 128])
    op("dve", "tensor_scalar", [io_f], [caus_f], caus_f[:, :], io_f[:, :], 0.0, NEG, ALU.is_gt, ALU.mult)
    maskT = [sb("maskT%d" % j, [128, 512], BF16) for j in range(4)]
    pow2 = sb("pow2", [128, NBIS + 1])
    S_.push()
    mk_i = sb("mk_i", [128, 512], I32)
    mk_f = sb("mk_f", [128, 512])
    op("pool", "iota", [], [mk_i], mk_i[:, :], [[1, 512]], base=0, channel_multiplier=-1)
    op("dve", "tensor_copy", [mk_i], [mk_f], mk_f[:, :], mk_i[:, :])
    for j in range(4):
        op("dve", "tensor_scalar", [mk_f], [maskT[j]], maskT[j][:, :], mk_f[:, :], float(128 * j), NEG, ALU.is_lt, ALU.mult)
    p2_i = sb("p2_i", [128, NBIS + 1], I32)
    p2_f = sb("p2_f", [128, NBIS + 1])
    op("pool", "iota", [], [p2_i], p2_i[:, :], [[1, NBIS + 1]], base=0, channel_multiplier=0)
    op("dve", "tensor_copy", [p2_i], [p2_f], p2_f[:, :], p2_i[:, :])
    op("act", "activation", [p2_f], [pow2], pow2[:, :], p2_f[:, :], AF.Exp, scale=-math.log(2.0))
    S_.pop()

    kk_sb = sb("kk2_sb", [128, S], BF16)
    va_sb = sb("va_sb", [128, NTS * 65], BF16)
    kbT_sb = sb("kbT_sb", [128, 8 * S], BF16)
    op("dve", "memset", [], [kbT_sb], kbT_sb[96:128, :], 0.0)
    vb_sb = sb("vb_sb", [128, NTS * 8 * 65], BF16)
    va3 = va_sb[:, :].rearrange("p (k c) -> p k c", c=65)
    kbT3 = kbT_sb[:, :].rearrange("p (h s) -> p h s", h=8)
    vb4 = vb_sb[:, :].rearrange("p (k h c) -> p k h c", h=8, c=65)
    qa_p = [sb("qa_p%d" % i, [128, 1024], BF16) for i in range(2)]
    qi_p = [sb("qi_p%d" % i, [128, 1024], BF16) for i in range(2)]
    for i in range(2):
        op("dve", "memset", [], [qa_p[i]], qa_p[i][64:128, :], 0.0)
        op("dve", "memset", [], [qi_p[i]], qi_p[i][0:64, :], 0.0)
    wi_t = [sb("wi_t%d" % i, [128, 8]) for i in range(2)]
    Dh2 = [sb("Dh0", [128, 1024], BF16)] * 2
    Rh = [sb("Rh%d" % i, [128, 512], BF16) for i in range(3)]
    score2 = [sb("score%d" % i, [128, S]) for i in range(2)]
    junk = sb("junk_dve", [128, S], mybir.dt.uint8)
    negm2 = [sb("negm%d" % i, [128, S], BF16) for i in range(2)]
    amax = sb("amax", [128, 1])
    s0 = sb("s0", [128, 1])
    hw = sb("hw", [128, NBIS + 1])
    mid = sb("mid", [128, 1])
    cnt = sb("cnt", [128, 1])
    dd = sb("dd", [128, 1])
    lo = sb("lo", [128, 1])
    PT = [sb("PT%d" % i, [128, 1024], BF16) for i in range(3)]
    oaT_s = [sb("oaT_s%d" % i, [64, 1024], BF16) for i in range(2)]
    qbT_h = [sb("qbT_h%d" % i, [128, 512], BF16) for i in range(3)]
    for i in range(3):
        op("dve", "memset", [], [qbT_h[i]], qbT_h[i][96:128, :], 0.0)
    obT_h = [sb("obT_h%d" % i, [64, 512], BF16) for i in range(2)]
    qhrot = [0]
    ohrot = [0]
    zrot = [0]
    ptrot = [0]
    rhrot = [0]

    deferred = []

    def flush():
        while deferred:
            deferred.pop(0)()

    oaU = [sb("oaU%d" % i, [64, 1024], BF16) for i in range(2)]
    lnr = [sb("lnr0", [65, 512])] * 2
    r_bf = [sb("r_bf0", [65, 1024], BF16)] * 2
    ones_bf = sb("ones_bf", [65, 64], BF16)
    op("dve", "memset", [], [ones_bf], ones_bf[:, :], 1.0)
    oarot = [0]

    def normalize(oacc_ap, oacc_res, width, dst_ap, dst_res, after=None):
        flush()
        i_ = oarot[0] % 2
        oarot[0] += 1
        ou, ln_, rb = oaU[i_], lnr[i_], r_bf[i_]
        for c in range(width // 512):
            op("act", "activation", oacc_res, [ln_], ln_[64:65, 0:512], oacc_ap[64:65, c * 512:(c + 1) * 512], AF.Ln)
            op("act", "activation", [ln_], [rb], rb[64:65, c * 512:(c + 1) * 512], ln_[64:65, 0:512], AF.Exp, scale=-1.0)
        op("act", "copy", oacc_res, [ou], ou[:, 0:width], oacc_ap[0:64, :])

        def partB():
            bcr = [pbank[6], pbank[7]]
            for c in range(width // 512):
                op("pe", "matmul", [ones_bf, rb], [bcr[c]], bcr[c][0:64, 0:512], ones_bf[64:65, 0:64], rb[64:65, c * 512:(c + 1) * 512],
                   start=True, stop=True)
            op("dve", "tensor_tensor", [ou] + bcr[0:width // 512], [dst_res], dst_ap, ou[:, 0:width], P2[3][0:64, 0:width], ALU.mult)
            if after is not None:
                after()
        deferred.append(partB)

    ntile_seq = [min(NTS, max(0, ntl - b * NTS)) for b in range(NB)]
    for b in range(NB):
        nts = ntile_seq[b]
        if nts == 0:
            continue
        nkeys = nts * 128
        S_.dma("sp", kk_sb[0:64, 0:nkeys], E.kaT_d[b, :, 0:nkeys], [E.kaT_d], [kk_sb], kk_sb)
        S_.dma("sp", kk_sb[64:128, 0:nkeys], E.kiT_d[b, :, 0:nkeys], [E.kiT_d], [kk_sb], kk_sb)
        S_.dma("sp", va3[:, 0:nts, :], E.va_d[b * S:b * S + nkeys, :].rearrange("(k p) c -> p k c", p=128), [E.va_d], [va_sb], va_sb)
        for h in range(8):
            S_.dma("sp", kbT3[0:96, h, 0:nkeys], E.kbT_d[b, :, h, 0:nkeys], [E.kbT_d], [kbT_sb], kbT_sb)
        S_.dma("sp", vb_sb[:, 0:nts * 520].rearrange("p (k c) -> p k c", c=520),
               E.vb_d[b * S:b * S + nkeys, :].rearrange("(k p) c -> p k c", p=128), [E.vb_d], [vb_sb], vb_sb)

        def qa_load(t):
            ti = b * NTS + t
            sl = t % 2
            S_.dma("sp", qa_p[sl][0:64, :].rearrange("p (h q) -> p h q", h=8), E.qaT_d[ti].rearrange("h d q -> d h q"), [E.qaT_d], [qa_p[sl]], qa_p[sl])

        def dsa_index(t):
            ti = b * NTS + t
            sl = t % 2
            score = score2[t % 2]
            nk = 128 * (t + 1)
            S_.dma("sp", qi_p[sl][64:128, :].rearrange("p (h q) -> p h q", h=8), E.qiT_d[ti].rearrange("h d q -> d h q"), [E.qiT_d], [qi_p[sl]], qi_p[sl])
            S_.dma("sp", wi_t[sl][:, :], E.wi_d[ti], [E.wi_d], [wi_t[sl]], wi_t[sl])
            Dh = Dh2[t % 2]
            op("pool", "tensor_tensor", [ident_f, wi_t[sl]], [Dh], Dh[:, :].rearrange("p (h q) -> p h q", h=8),
               ident_f[:, :].unsqueeze(1).broadcast_to([128, 8, 128]), wi_t[sl][:, :].unsqueeze(2).broadcast_to([128, 8, 128]), ALU.mult)
            nch = (nk + 511) // 512
            items = [(c, h) for c in range(nch) for h in range(8)]

            def zmm(c, h):
                w = min(512, nk - 512 * c)
                zb = pbank[zrot[0] % 4]
                zrot[0] += 1
                op("pe", "matmul", [qi_p[sl], kk_sb], [zb], zb[:, 0:w], qi_p[sl][:, h * 128:(h + 1) * 128], kk_sb[:, 512 * c:512 * c + w],
                   start=True, stop=True)
                return zb

            znext = zmm(*items[0])
            for i, (c, h) in enumerate(items):
                w = min(512, nk - 512 * c)
                zb = znext
                if i + 1 < len(items):
                    znext = zmm(*items[i + 1])
                sc = pbank[6 + (c % 2)]
                rh = Rh[rhrot[0] % 3]
                rhrot[0] += 1
                op("act", "activation", [zb], [rh], rh[:, 0:w], zb[:, 0:w], AF.Relu)
                op("pe", "matmul", [Dh, rh], [sc], sc[:, 0:w], Dh[:, h * 128:(h + 1) * 128], rh[:, 0:w], start=(h == 0), stop=(h == 7))
                if h == 7:
                    op("act", "copy", [sc], [score], score[:, 512 * c:512 * c + w], sc[:, 0:w])
                if h == 7 and c == 0:
                    flush()

        def dsa_bisect(t):
            nk = 128 * (t + 1)
            score = score2[t % 2]
            negm = negm2[t % 2]
            op("dve", "tensor_reduce", [score], [amax], amax[:, :], score[:, 0:nk], AX.X, ALU.max, apply_absolute_value=True)
            op("dve", "tensor_tensor", [score, caus_f], [score], score[:, nk - 128:nk], score[:, nk - 128:nk], caus_f[:, :], ALU.add)
            op("dve", "tensor_scalar", [amax], [s0], s0[:, :], amax[:, :], 1.001, 1e-6, ALU.mult, ALU.add)
            op("dve", "tensor_scalar", [pow2, s0], [hw], hw[:, :], pow2[:, :], s0[:, 0:1], None, ALU.mult)
            op("dve", "memset", [], [mid], mid[:, :], 0.0)
            for k in range(NBIS):
                op("dve", "tensor_scalar", [score, mid], [junk, cnt], junk[:, 0:nk], score[:, 0:nk], mid[:, 0:1], 0.0, ALU.is_ge, ALU.add,
                   accum_out=cnt[:, 0:1])
                op("dve", "tensor_scalar", [cnt, hw], [dd], dd[:, :], cnt[:, :], 255.5, hw[:, k:k + 1], ALU.is_ge, ALU.mult)
                op("dve", "scalar_tensor_tensor", [dd, hw, mid], [mid], mid[:, :], dd[:, :], hw[:, k + 1:k + 2], mid[:, :], ALU.subtract, ALU.add)
            op("dve", "tensor_scalar", [mid, hw], [lo], lo[:, :], mid[:, :], hw[:, NBIS:NBIS + 1], None, ALU.subtract)
            op("dve", "tensor_scalar", [score, lo], [negm], negm[:, 0:nk], score[:, 0:nk], lo[:, 0:1], NEG, ALU.is_lt, ALU.mult)
            if "dbg_negm" in E.debug:
                ti = b * NTS + t
                S_.dma("pool", E.dbg_negm[ti, :, 0:nk], negm[:, 0:nk], [negm], [E.dbg_negm], negm)
                S_.dma("pool", E.dbg_score[ti, :, 0:nk], score[:, 0:nk], [score], [E.dbg_score], score)

        def dsa_attn(t):
            ti = b * NTS + t
            sl = t % 2
            negm = negm2[t % 2]
            oacc = P2[2]
            oacc_res = [pbank[4], pbank[5]]

            def qk(kb):
                zi = (zrot[0] % 2)
                zrot[0] += 1
                z = P2[zi]
                zres = [pbank[2 * zi], pbank[2 * zi + 1]]
                for c in range(2):
                    op("pe", "matmul", [kk_sb, qa_p[sl]], [zres[c]], z[:, c * 512:(c + 1) * 512], kk_sb[:, kb * 128:(kb + 1) * 128],
                       qa_p[sl][:, c * 512:(c + 1) * 512], start=True, stop=False)
                    op("pe", "matmul", [negm, ident8], [zres[c]], z[:, c * 512:(c + 1) * 512], negm[:, kb * 128:(kb + 1) * 128],
                       ident8[:, c * 512:(c + 1) * 512], start=False, stop=True)
                return z, zres

            nxt = qk(0)
            for kb in range(t + 1):
                z, zres = nxt
                if kb + 1 <= t:
                    nxt = qk(kb + 1)
                pt = PT[ptrot[0] % 3]
                ptrot[0] += 1
                op("act", "activation", zres, [pt], pt[:, :], z[:, :], AF.Exp, scale=0.125)
                for c in range(2):
                    op("pe", "matmul", [va_sb, pt], [oacc_res[c]], oacc[0:65, c * 512:(c + 1) * 512], va3[:, kb, :], pt[:, c * 512:(c + 1) * 512],
                       start=(kb == 0), stop=(kb == t))
                if kb == 1:
                    flush()

            def store():
                S_.dma("pool", E.oaT_d[ti].rearrange("d h q -> d (h q)"), oaT_s[sl][:, :], [oaT_s[sl]], [E.oaT_d], oaT_s[sl])
            normalize(oacc[0:65, :], oacc_res, 1024, oaT_s[sl][:, :], oaT_s[sl], after=store)

        def mla_heads(g, heads, ntg):
            qh = {}
            for h in heads:
                qt = qbT_h[qhrot[0] % 3]
                qhrot[0] += 1
                S_.dma("sp", qt[0:96, :], E.qbT_d[b, :, h, 512 * g:512 * g + 512], [E.qbT_d], [qt], qt)
                qh[h] = qt
            nkb = min(4 * g + 4, nts)
            units = [(h, kb) for h in heads for kb in range(nkb)]

            def qk(h, kb):
                zb = pbank[zrot[0] % 4]
                zrot[0] += 1
                diag = kb >= 4 * g
                op("pe", "matmul", [kbT_sb, qh[h]], [zb], zb[:, 0:512], kbT3[:, h, kb * 128:(kb + 1) * 128], qh[h][:, :],
                   start=True, stop=(not diag))
                if diag:
                    op("pe", "matmul", [ident_bf, maskT[kb - 4 * g]], [zb], zb[:, 0:512], ident_bf[:, :], maskT[kb - 4 * g][:, :],
                       start=False, stop=True)
                return zb

            nxt = qk(*units[0])
            for i, (h, kb) in enumerate(units):
                zb = nxt
                if i + 1 < len(units):
                    nxt = qk(*units[i + 1])
                oacc = pbank[4 + (h % 2)]
                pt = PT[ptrot[0] % 3]
                ptrot[0] += 1
                op("act", "activation", [zb], [pt], pt[:, 0:512], zb[:, 0:512], AF.Exp, scale=96.0 ** -0.5)
                op("pe", "matmul", [vb_sb, pt], [oacc], oacc[0:65, 0:512], vb4[:, kb, h, :], pt[:, 0:512], start=(kb == 0), stop=(kb == nkb - 1))
                if kb == min(2, nkb - 1):
                    flush()
                if kb == nkb - 1:
                    ot = obT_h[ohrot[0] % 2]
                    ohrot[0] += 1

                    def store(ot=ot, h=h):
                        ti0 = b * NTS + 4 * g
                        S_.dma("pool", E.obT_d[ti0:ti0 + ntg, :, h, :].rearrange("t d q -> d t q"),
                               ot[:, 0:ntg * 128].rearrange("p (t q) -> p t q", q=128), [ot], [E.obT_d], ot)
                    normalize(oacc[0:65, 0:512], [oacc], 512, ot[:, :], ot, after=store)

        qa_load(0)
        dsa_index(0)
        dsa_bisect(0)
        if nts > 1:
            dsa_index(1)
        for t in range(nts):
            g, j = divmod(t, 4)
            ntg = min(4, nts - 4 * g)
            if t + 1 < nts:
                qa_load(t + 1)
            if t + 2 < nts:
                dsa_index(t + 2)
            if t + 1 < nts:
                dsa_bisect(t + 1)
            hs = [2 * j, 2 * j + 1]
            if j == ntg - 1:
                hs = list(range(2 * j, 8))
            mla_heads(g, hs, ntg)
            dsa_attn(t)
        flush()
    S_.pop()


def phase3(S_, envd):
    E = Env(envd)
    op = S_.op
    sb, pbank, P2 = E.sb, E.pbank, E.P2
    ident_f, ident_bf, ones_f = E.ident_f, E.ident_bf, E.ones_f
    rstd_from_ss, rms_heads, bcast_load, junk_act = E.rstd_from_ss, E.rms_heads, E.bcast_load, E.junk_act
    ntl = E.ntl
    S_.push()

    def pbf(i):
        return pbank[i][:, :].bitcast(BF16)

    wbrA = sb("wbrA", [128, 4 * D], BF16)
    wbrB = sb("wbrB", [128, 4 * D], BF16)
    wout = sb("wout", [128, 8 * D], BF16)
    wq_m = sb("wq_m", [128, 8 * 256], BF16)
    wkv_m = sb("wkv_m", [128, 8 * 512], BF16)
    wo_m = sb("wo_m", [64, 4 * D], BF16)
    wbrA3 = wbrA[:, :].rearrange("p (h n) -> p h n", h=4)
    wbrB3 = wbrB[:, :].rearrange("p (h n) -> p h n", h=4)
    wout3 = wout[:, :].rearrange("p (k n) -> p k n", k=8)
    wq3 = wq_m[:, :].rearrange("p (k n) -> p k n", k=8)
    wkv3 = wkv_m[:, :].rearrange("p (k n) -> p k n", k=8)
    wo3 = wo_m[:, :].rearrange("p (h n) -> p h n", h=4)
    for h in range(8):
        if h < 4:
            S_.dma("pool", wbrA3[:, h, :], E.w_bra_d[h * 128:(h + 1) * 128, :], [E.w_bra_d], [wbrA], wbrA)
            S_.dma("pool", wbrB3[:, h, :], E.w_brb_d[h * 128:(h + 1) * 128, :], [E.w_brb_d], [wbrB], wbrB)
        S_.dma("pool", wout3[:, h, :], E.w_out_d[h * 128:(h + 1) * 128, :], [E.w_out_d], [wout], wout)
        S_.dma("pool", wq3[:, h, :], E.mem_wq_d[h * 128:(h + 1) * 128, :], [E.mem_wq_d], [wq_m], wq_m)
        S_.dma("pool", wkv3[:, h, :], E.mem_wkv_d[h * 128:(h + 1) * 128, :], [E.mem_wkv_d], [wkv_m], wkv_m)
    for h in range(4):
        S_.dma("pool", wo3[:, h, :], E.mem_wo_d[h * 64:(h + 1) * 64, :], [E.mem_wo_d], [wo_m], wo_m)
    g_memx = bcast_load("g_memx", E.memx_g_d, D)
    g_mem = bcast_load("g_mem", E.mem_g_d, D)
    g_mq = bcast_load("g_memq", E.memq_g_d, 64)
    g_mk = bcast_load("g_memk", E.memk_g_d, 64)

    xin = [sb("x3in%d" % i, [128, D]) for i in range(2)]
    oaT = [sb("oaT_l%d" % i, [128, 512], BF16) for i in range(2)]
    obT = [sb("obT_l%d" % i, [128, 512], BF16) for i in range(2)]
    sg = [sb("sg_l%d" % i, [128, 2048], BF16) for i in range(2)]
    mg_f = sb("mg_f", [128, D])
    mg_t = sb("mg_t", [128, D])
    mg_bf = sb("mg_bf", [128, D], BF16)
    mT = sb("mT", [128, D], BF16)
    x1b = [sb("x1_%d" % i, [128, D]) for i in range(2)]
    ss = sb("ss3", [128, 16])
    r1 = sb("r13", [128, 16])
    rstd = sb("rstd3", [128, 16])
    h2 = sb("h2", [128, D], BF16)
    h2T = sb("h2T", [128, D], BF16)
    qm_f = sb("qm_f", [128, 256])
    qm_n = sb("qm_n", [128, 256])
    qm_sb = sb("qm_sb", [128, 256], BF16)
    qmT = sb("qmT", [64, 512], BF16)
    t_sq = sb("t3_sq", [128, 256])
    t_xn = sb("t3_xn", [128, 256])
    t_ssq = sb("t3_ssq", [128, 16])
    t_r1 = sb("t3_r1", [128, 16])
    t_rstd = sb("t3_rstd", [128, 16])
    tmps = (t_sq, t_ssq, t_r1, t_rstd, t_xn)
    kmT = sb("kmT", [64, 4 * 256], BF16)
    vm = sb("vm", [128, 2 * 4 * 65], BF16)
    kmT3 = kmT[:, :].rearrange("p (h m) -> p h m", h=4)
    vm4 = vm[:, :].rearrange("p (b h c) -> p b h c", b=2, h=4)
    memt = sb("memt", [128, D])
    mn = sb("mn", [128, D], BF16)
    mnT = sb("mnT", [128, D], BF16)
    kvm_f = sb("kvm_f", [128, 512])
    km_n = sb("km_n", [128, 256])
    km_sb = sb("km_sb", [128, 256], BF16)
    PTm = [sb("PTm%d" % i, [128, 512], BF16) for i in range(2)]
    oa_sb = sb("oa3_sb", [65, 512])
    r3_bf = sb("r3_bf", [65, 512], BF16)
    oaU3 = sb("oaU3", [64, 512], BF16)
    ones3_bf = sb("ones3_bf", [65, 64], BF16)
    op("dve", "memset", [], [ones3_bf], ones3_bf[:, :], 1.0)
    omT = sb("omT", [64, 512], BF16)
    x2s = [sb("x2s%d" % i, [128, D]) for i in range(2)]
    op("dve", "memset", [], [vm], vm[:, :], 1.0)
    zr = [0]

    def zbank():
        z = pbank[zr[0] % 4]
        zr[0] += 1
        return z

    zrA = [0]
    zrB = [0]

    def zbankA():
        z = pbank[zrA[0] % 2]
        zrA[0] += 1
        return z

    def zbankB():
        z = pbank[2 + zrB[0] % 2]
        zrB[0] += 1
        return z

    def evac_copy(dst_ap, dst_res, src_ap, src_res, flip=[0]):
        if flip[0] % 2 == 0:
            op("act", "copy", [src_res], [dst_res], dst_ap, src_ap)
        else:
            op("dve", "tensor_copy", [src_res], [dst_res], dst_ap, src_ap)
        flip[0] += 1

    def norm_to_bf(src, gbc, dst):
        op("act", "activation", [src], [ss], junk_act[:, :], src[:, :], AF.Square, accum_out=ss[:, 0:1])
        rstd_from_ss(ss, rstd, 1, D, r1)
        op("dve", "scalar_tensor_tensor", [src, rstd, gbc], [dst], dst[:, :], src[:, :], rstd[:, 0:1], gbc[:, :], ALU.mult, ALU.mult)

    def transpose8(src_bf, dstT, bank):
        for kc in range(8):
            op("pe", "transpose", [src_bf, ident_bf], [pbank[bank]], pbf(bank)[:, kc * 128:(kc + 1) * 128], src_bf[:, kc * 128:(kc + 1) * 128], ident_bf[:, :])
        op("act", "copy", [pbank[bank]], [dstT], dstT[:, :], pbf(bank)[:, :])

    ntile_seq = [min(NTS, max(0, ntl - b * NTS)) for b in range(NB)]
    for b in range(NB):
        if ntile_seq[b] == 0:
            continue
        for mb in range(2):
            S_.dma("sp", memt[:, :], E.mem_d[b * 256 + mb * 128:b * 256 + (mb + 1) * 128, :], [E.mem_d], [memt], memt)
            norm_to_bf(memt, g_mem, mn)
            transpose8(mn, mnT, 4)
            zb = zbank()
            for kc in range(8):
                op("pe", "matmul", [mnT, wkv_m], [zb], zb[:, 0:512], mnT[:, kc * 128:(kc + 1) * 128], wkv3[:, kc, :], start=(kc == 0), stop=(kc == 7))
            op("act", "copy", [zb], [kvm_f], kvm_f[:, :], zb[:, 0:512])
            rms_heads(kvm_f[:, 0:256].rearrange("p (h d) -> p h d", h=4), kvm_f, km_n[:, :].rearrange("p (h d) -> p h d", h=4), km_n, 4, 64, g_mk, tmps)
            op("dve", "tensor_copy", [km_n], [km_sb], km_sb[:, :], km_n[:, :])
            for h in range(4):
                op("pe", "transpose", [km_sb, ident_bf], [pbank[5]], pbf(5)[0:64, h * 128:(h + 1) * 128], km_sb[:, h * 64:(h + 1) * 64], ident_bf[:, :])
            op("act", "copy", [pbank[5]], [kmT], kmT3[:, :, mb * 128:(mb + 1) * 128], pbf(5)[0:64, 0:512].rearrange("p (h m) -> p h m", h=4))
            op("dve", "tensor_copy", [kvm_f], [vm], vm4[:, mb, :, 0:64], kvm_f[:, 256:512].rearrange("p (h d) -> p h d", h=4))
        def stA(t):
            ti = b * NTS + t
            sl = ti % 2
            xt = xin[sl]
            x1 = x1b[ti % 2]
            S_.dma("sp", xt[:, :], E.x_d[ti * 128:(ti + 1) * 128, :], [E.x_d], [xt], xt)
            for hh in range(2):
                S_.dma("sp", oaT[sl][hh * 64:(hh + 1) * 64, :].rearrange("p (j q) -> p j q", j=4),
                       E.oaT_d[ti].rearrange("d (j hh) q -> hh d j q", hh=2)[hh], [E.oaT_d], [oaT[sl]], oaT[sl])
                S_.dma("sp", obT[sl][hh * 64:(hh + 1) * 64, :].rearrange("p (j q) -> p j q", j=4),
                       E.obT_d[ti].rearrange("d (j hh) q -> hh d j q", hh=2)[hh], [E.obT_d], [obT[sl]], obT[sl])
            S_.dma("sp", sg[sl][:, :], E.sg_d[ti], [E.sg_d], [sg[sl]], sg[sl])
            for (src, w3, goff, first) in ((oaT[sl], wbrA3, 0, True), (obT[sl], wbrB3, 1024, False)):
                wres = wbrA if first else wbrB
                for nchunk in range(2):
                    zb = zbankA()
                    for h in range(4):
                        op("pe", "matmul", [src, wres], [zb], zb[:, 0:512], src[:, h * 128:(h + 1) * 128], w3[:, h, nchunk * 512:(nchunk + 1) * 512],
                           start=(h == 0), stop=(h == 3))
                    dst = mg_f if first else mg_t
                    op("dve", "tensor_tensor", [zb, sg[sl]], [dst], dst[:, nchunk * 512:(nchunk + 1) * 512], zb[:, 0:512],
                       sg[sl][:, goff + nchunk * 512:goff + (nchunk + 1) * 512], ALU.mult)
                    yield
            op("pool", "tensor_tensor", [mg_f, mg_t], [mg_bf], mg_bf[:, :], mg_f[:, :], mg_t[:, :], ALU.add)
            yield
            transpose8(mg_bf, mT, 4)
            yield
            for nchunk in range(2):
                zb = zbankA()
                for kc in range(8):
                    op("pe", "matmul", [mT, wout], [zb], zb[:, 0:512], mT[:, kc * 128:(kc + 1) * 128], wout3[:, kc, nchunk * 512:(nchunk + 1) * 512],
                       start=(kc == 0), stop=(kc == 7))
                op("dve", "tensor_tensor", [zb, xt], [x1], x1[:, nchunk * 512:(nchunk + 1) * 512], zb[:, 0:512], xt[:, nchunk * 512:(nchunk + 1) * 512], ALU.add)
                yield

        def stB(t):
            ti = b * NTS + t
            sl = ti % 2
            x1 = x1b[ti % 2]
            norm_to_bf(x1, g_memx, h2)
            yield
            transpose8(h2, h2T, 5)
            yield
            zb = zbankB()
            for kc in range(8):
                op("pe", "matmul", [h2T, wq_m], [zb], zb[:, 0:256], h2T[:, kc * 128:(kc + 1) * 128], wq3[:, kc, :], start=(kc == 0), stop=(kc == 7))
            op("act", "copy", [zb], [qm_f], qm_f[:, :], zb[:, 0:256])
            yield
            rms_heads(qm_f[:, :].rearrange("p (h d) -> p h d", h=4), qm_f, qm_n[:, :].rearrange("p (h d) -> p h d", h=4), qm_n, 4, 64, g_mq, tmps)
            op("dve", "tensor_copy", [qm_n], [qm_sb], qm_sb[:, :], qm_n[:, :])
            yield
            for h in range(4):
                op("pe", "transpose", [qm_sb, ident_bf], [pbank[5]], pbf(5)[0:64, h * 128:(h + 1) * 128], qm_sb[:, h * 64:(h + 1) * 64], ident_bf[:, :])
            op("act", "copy", [pbank[5]], [qmT], qmT[:, :], pbf(5)[0:64, 0:512])
            yield
            for mb in range(2):
                zb = zbankB()
                for h in range(4):
                    op("pe", "matmul", [kmT, qmT], [zb], zb[:, h * 128:(h + 1) * 128], kmT3[:, h, mb * 128:(mb + 1) * 128], qmT[:, h * 128:(h + 1) * 128],
                       start=True, stop=True)
                op("act", "activation", [zb], [PTm[mb]], PTm[mb][:, :], zb[:, 0:512], AF.Exp, scale=0.125)
            yield
            oacc = pbank[6]
            for h in range(4):
                for mb in range(2):
                    op("pe", "matmul", [vm, PTm[mb]], [oacc], oacc[0:65, h * 128:(h + 1) * 128], vm4[:, mb, h, :], PTm[mb][:, h * 128:(h + 1) * 128],
                       start=(mb == 0), stop=(mb == 1))
            op("act", "activation", [oacc], [oa_sb], oa_sb[64:65, :], oacc[64:65, 0:512], AF.Ln)
            op("act", "activation", [oa_sb], [r3_bf], r3_bf[64:65, :], oa_sb[64:65, :], AF.Exp, scale=-1.0)
            op("act", "copy", [oacc], [oaU3], oaU3[:, :], oacc[0:64, 0:512])
            yield
            op("pe", "matmul", [ones3_bf, r3_bf], [pbank[7]], pbank[7][0:64, 0:512], ones3_bf[64:65, 0:64], r3_bf[64:65, :], start=True, stop=True)
            op("dve", "tensor_tensor", [oaU3, pbank[7]], [omT], omT[:, :], oaU3[:, :], pbank[7][0:64, 0:512], ALU.mult)
            yield
            x2t = x2s[sl]
            for nchunk in range(2):
                zb = zbankB()
                for h in range(4):
                    op("pe", "matmul", [omT, wo_m], [zb], zb[:, 0:512], omT[:, h * 128:(h + 1) * 128], wo3[:, h, nchunk * 512:(nchunk + 1) * 512],
                       start=(h == 0), stop=(h == 3))
                op("dve", "tensor_tensor", [zb, x1], [x2t], x2t[:, nchunk * 512:(nchunk + 1) * 512], zb[:, 0:512], x1[:, nchunk * 512:(nchunk + 1) * 512], ALU.add)
            S_.dma("pool", E.x2_d[ti * 128:(ti + 1) * 128, :], x2t[:, :], [x2t], [E.x2_d], x2t)

        nts_ = ntile_seq[b]

        def interleave(gens):
            alive = list(gens)
            while alive:
                for g_ in list(alive):
                    try:
                        next(g_)
                    except StopIteration:
                        alive.remove(g_)

        interleave([stA(0)])
        for t in range(nts_):
            gs_ = [stB(t)]
            if t + 1 < nts_:
                gs_.insert(0, stA(t + 1))
            interleave(gs_)
    S_.pop()


def phase4(S_, envd):
    E = Env(envd)
    op = S_.op
    sb, pbank, P2 = E.sb, E.pbank, E.P2
    ident_f, ident_bf = E.ident_f, E.ident_bf
    rstd_from_ss, bcast_load, junk_act = E.rstd_from_ss, E.bcast_load, E.junk_act
    ntl = E.ntl
    S_.push()

    def pbf(i):
        return pbank[i][:, :].bitcast(BF16)

    wd = sb("wd", [128, 16 * 2 * D], BF16)
    wd4 = wd[:, :].rearrange("p (e f n) -> p e f n", e=16, f=2)
    for e in range(16):
        S_.dma("pool", wd4[:, e, :, :], E.moe_down_d[e].rearrange("(f p) n -> p f n", p=128), [E.moe_down_d], [wd], wd)
    wr = sb("wr", [128, 8 * 20], BF16)
    wr3 = wr[:, :].rearrange("p (k n) -> p k n", k=8)
    S_.dma("pool", wr3[:, :, 0:4], E.moe_wg_d[:, :].rearrange("(k p) n -> p k n", p=128), [E.moe_wg_d], [wr], wr)
    S_.dma("pool", wr3[:, :, 4:20], E.moe_we_d[:, :].rearrange("(k p) n -> p k n", p=128), [E.moe_we_d], [wr], wr)
    g_moe = bcast_load("g_moe", E.moe_g_d, D)
    bias_bc = bcast_load("bias_bc", E.moe_b_d, 16)
    sel = sb("sel_e", [16, 16 * 128], BF16)
    op("dve", "tensor_copy", [ident_f], [sel], sel[:, :].rearrange("p (e m) -> p e m", e=16),
       ident_f[0:16, 0:16].unsqueeze(2).broadcast_to([16, 16, 128]))

    x2g2 = [sb("x2g%d" % i, [128, 4 * D]) for i in range(2)]
    h3 = sb("h3", [128, D], BF16)
    h3T2 = [sb("h3T%d" % i, [128, 8 * 512], BF16) for i in range(2)]
    ss = sb("ss4", [128, 16])
    r1 = sb("r14", [128, 16])
    rstd = sb("rstd4", [128, 16])
    lg = sb("lg", [128, 20])
    sm = {n: sb("r_" + n, [128, 16]) for n in ("nlmax", "e4", "esum", "pg", "oh", "aff", "bi", "m1", "k1", "bi2", "m2", "k2", "sel", "asel", "den", "fac", "comb")}
    cT2 = [sb("cT%d" % i, [16, 512]) for i in range(2)]
    cH2 = [sb("cH%d" % i, [16, 512], BF16) for i in range(2)]
    cL2 = [sb("cL%d" % i, [16, 512], BF16) for i in range(2)]
    comb4 = [sb("comb4_%d" % i, [128, 16]) for i in range(4)]
    cbc = sb("cbc", [128, 512])
    sgl = [sb("sgl%d" % i, [128, 512], BF16) for i in range(2)]
    tu = [sb("tu%d" % i, [128, 512], BF16) for i in range(2)]
    actc = sb("actc", [128, 16 * 2 * 512], BF16)
    actc4 = actc[:, :].rearrange("p (e f q) -> p e f q", e=16, f=2)
    wgu = [sb("wgu%d" % i, [128, 8 * 512], BF16) for i in range(3)]
    o_s = [sb("o_s%d" % i, [128, D]) for i in range(2)]
    zr = [0]

    def zbank():
        z = pbank[zr[0] % 4]
        zr[0] += 1
        return z

    ngr = (ntl + 3) // 4
    wrot = [0]
    orot = [0]
    def pre_(g):
        ntg = min(4, ntl - 4 * g)
        W = ntg * 128
        x2g = x2g2[g % 2]
        h3T = h3T2[g % 2]
        h3T3 = h3T[:, :].rearrange("p (k q) -> p k q", k=8)
        cT = cT2[g % 2]
        cH = cH2[g % 2]
        cL = cL2[g % 2]
        for j in range(ntg):
            ti = 4 * g + j
            xs_ = x2g[:, j * D:(j + 1) * D]
            S_.dma("sp", xs_, E.x2_d[ti * 128:(ti + 1) * 128, :], [E.x2_d], [x2g], x2g)
        yield
        for j in range(ntg):
            xs_ = x2g[:, j * D:(j + 1) * D]
            op("act", "activation", [x2g], [ss], junk_act[:, :], xs_, AF.Square, accum_out=ss[:, 0:1])
            rstd_from_ss(ss, rstd, 1, D, r1)
            op("dve", "scalar_tensor_tensor", [x2g, rstd, g_moe], [h3], h3[:, :], xs_, rstd[:, 0:1], g_moe[:, :], ALU.mult, ALU.mult)
            yield
            for kc in range(8):
                op("pe", "transpose", [h3, ident_bf], [pbank[4]], pbf(4)[:, kc * 128:(kc + 1) * 128], h3[:, kc * 128:(kc + 1) * 128], ident_bf[:, :])
            op("act", "copy", [pbank[4]], [h3T], h3T3[:, :, j * 128:(j + 1) * 128], pbf(4)[:, :].rearrange("p (k q) -> p k q", k=8))
            rb = pbank[5]
            for kc in range(8):
                op("pe", "matmul", [h3T, wr], [rb], rb[:, 0:20], h3T3[:, kc, j * 128:(j + 1) * 128], wr3[:, kc, :], start=(kc == 0), stop=(kc == 7))
            op("dve", "tensor_copy", [rb], [lg], lg[:, :], rb[:, 0:20])
            yield
            m = sm
            op("dve", "tensor_reduce", [lg], [m["nlmax"]], m["nlmax"][:, 0:1], lg[:, 0:4], AX.X, ALU.max, negate=True)
            op("act", "activation", [lg, m["nlmax"]], [m["e4"], m["esum"]], m["e4"][:, 0:4], lg[:, 0:4], AF.Exp, bias=m["nlmax"][:, 0:1],
               accum_out=m["esum"][:, 0:1])
            op("dve", "reciprocal", [m["esum"]], [m["pg"]], m["pg"][:, 0:1], m["esum"][:, 0:1])
            op("dve", "tensor_scalar", [lg, m["nlmax"]], [m["oh"]], m["oh"][:, 0:4], lg[:, 0:4], m["nlmax"][:, 0:1], 0.0, ALU.add, ALU.is_ge)
            op("act", "activation", [lg], [m["aff"]], m["aff"][:, :], lg[:, 4:20], AF.Sigmoid)
            op("dve", "tensor_tensor", [m["aff"], bias_bc], [m["bi"]], m["bi"][:, :], m["aff"][:, :], bias_bc[:, :], ALU.add)
            v3 = lambda n: m[n][:, :].rearrange("p (g e) -> p g e", g=4)
            b4 = lambda n: m[n][:, 0:4].unsqueeze(2).broadcast_to([128, 4, 4])
            op("dve", "tensor_reduce", [m["bi"]], [m["m1"]], m["m1"][:, 0:4], v3("bi"), AX.X, ALU.max)
            op("dve", "tensor_tensor", [m["bi"], m["m1"]], [m["k1"]], v3("k1"), v3("bi"), b4("m1"), ALU.is_ge)
            op("dve", "scalar_tensor_tensor", [m["k1"], m["bi"]], [m["bi2"]], m["bi2"][:, :], m["k1"][:, :], -1e9, m["bi"][:, :], ALU.mult, ALU.add)
            op("dve", "tensor_reduce", [m["bi2"]], [m["m2"]], m["m2"][:, 0:4], v3("bi2"), AX.X, ALU.max)
            op("dve", "tensor_tensor", [m["bi2"], m["m2"]], [m["k2"]], v3("k2"), v3("bi2"), b4("m2"), ALU.is_ge)
            op("dve", "tensor_tensor", [m["k1"], m["k2"]], [m["sel"]], m["sel"][:, :], m["k1"][:, :], m["k2"][:, :], ALU.add)
            op("dve", "tensor_tensor", [m["sel"], m["oh"]], [m["sel"]], v3("sel"), v3("sel"), b4("oh"), ALU.mult)
            op("dve", "tensor_tensor", [m["aff"], m["sel"]], [m["asel"]], m["asel"][:, :], m["aff"][:, :], m["sel"][:, :], ALU.mult)
            op("dve", "tensor_reduce", [m["asel"]], [m["den"]], m["den"][:, 0:1], m["asel"][:, :], AX.X, ALU.add)
            op("dve", "reciprocal", [m["den"]], [m["den"]], m["den"][:, 0:1], m["den"][:, 0:1])
            op("dve", "tensor_tensor", [m["den"], m["pg"]], [m["fac"]], m["fac"][:, 0:1], m["den"][:, 0:1], m["pg"][:, 0:1], ALU.mult)
            op("dve", "tensor_scalar", [m["asel"], m["fac"]], [m["comb"]], m["comb"][:, :], m["asel"][:, :], m["fac"][:, 0:1], None, ALU.mult)
            op("pool", "tensor_copy", [m["comb"]], [comb4[j]], comb4[j][:, :], m["comb"][:, :])
            yield
        for j in range(ntg):
            op("pe", "transpose", [comb4[j], ident_f], [pbank[6]], pbank[6][0:16, j * 128:(j + 1) * 128], comb4[j][:, :], ident_f[:, :])
        op("dve", "tensor_copy", [pbank[6]], [cT], cT[:, 0:W], pbank[6][0:16, 0:W])
        op("dve", "tensor_copy", [cT], [cH], cH[:, 0:W], cT[:, 0:W])
        op("dve", "tensor_tensor", [cT, cH], [cL], cL[:, 0:W], cT[:, 0:W], cH[:, 0:W], ALU.subtract)

    def experts_(g, gen=None):
        ntg = min(4, ntl - 4 * g)
        W = ntg * 128
        x2g = x2g2[g % 2]
        h3T = h3T2[g % 2]
        h3T3 = h3T[:, :].rearrange("p (k q) -> p k q", k=8)
        cT = cT2[g % 2]
        cH = cH2[g % 2]
        cL = cL2[g % 2]
        for e in range(16):
            wt = wgu[wrot[0] % 3]
            wrot[0] += 1
            wt3 = wt[:, :].rearrange("p (k f) -> p k f", k=8)
            S_.dma("sp", wt3, E.wgu_d[e], [E.wgu_d], [wt], wt)
            cb = pbank[7]
            op("pe", "matmul", [sel, cH], [cb], cb[:, 0:W], sel[:, e * 128:(e + 1) * 128], cH[:, 0:W], start=True, stop=False)
            op("pe", "matmul", [sel, cL], [cb], cb[:, 0:W], sel[:, e * 128:(e + 1) * 128], cL[:, 0:W], start=False, stop=True)
            op("act", "copy", [cb], [cbc], cbc[:, 0:W], cb[:, 0:W])
            for fc in range(2):
                pg_ = zbank()
                for kc in range(8):
                    op("pe", "matmul", [wt, h3T], [pg_], pg_[:, 0:W], wt3[:, kc, fc * 128:(fc + 1) * 128], h3T3[:, kc, 0:W], start=(kc == 0), stop=(kc == 7))
                pu_ = zbank()
                for kc in range(8):
                    op("pe", "matmul", [wt, h3T], [pu_], pu_[:, 0:W], wt3[:, kc, 256 + fc * 128:256 + (fc + 1) * 128], h3T3[:, kc, 0:W],
                       start=(kc == 0), stop=(kc == 7))
                op("act", "activation", [pg_], [sgl[fc]], sgl[fc][:, 0:W], pg_[:, 0:W], AF.Silu)
                op("dve", "tensor_tensor", [pu_, cbc], [tu[fc]], tu[fc][:, 0:W], pu_[:, 0:W], cbc[:, 0:W], ALU.mult)
                op("pool", "tensor_tensor", [sgl[fc], tu[fc]], [actc], actc4[:, e, fc, 0:W], sgl[fc][:, 0:W], tu[fc][:, 0:W], ALU.mult)
            if gen is not None:
                next(gen, None)

    def down_(g):
        ntg = min(4, ntl - 4 * g)
        W = ntg * 128
        x2g = x2g2[g % 2]
        h3T = h3T2[g % 2]
        h3T3 = h3T[:, :].rearrange("p (k q) -> p k q", k=8)
        cT = cT2[g % 2]
        cH = cH2[g % 2]
        cL = cL2[g % 2]
        for j in range(ntg):
            ti = 4 * g + j
            ot = o_s[orot[0] % 2]
            orot[0] += 1
            for nchunk in range(2):
                zb = pbank[4 + nchunk]
                for e in range(16):
                    for fc in range(2):
                        op("pe", "matmul", [actc, wd], [zb], zb[:, 0:512], actc4[:, e, fc, j * 128:(j + 1) * 128], wd4[:, e, fc, nchunk * 512:(nchunk + 1) * 512],
                           start=(e == 0 and fc == 0), stop=(e == 15 and fc == 1))
                op("dve", "tensor_tensor", [zb, x2g], [ot], ot[:, nchunk * 512:(nchunk + 1) * 512], zb[:, 0:512],
                   x2g[:, j * D + nchunk * 512:j * D + (nchunk + 1) * 512], ALU.add)
            S_.dma("pool", E.out_d[ti * 128:(ti + 1) * 128, :], ot[:, :], [ot], [E.out_d], ot)

    for _ in pre_(0):
        pass
    for g in range(ngr):
        gen = pre_(g + 1) if g + 1 < ngr else None
        experts_(g, gen)
        if gen is not None:
            for _ in gen:
                pass
        down_(g)
    S_.pop()


_INPUT_ORDER = None


def kernel(**inputs):
    nc, stack = build()
    in_maps = make_in_maps(inputs)
    res = run_bass_kernel_spmd(nc, in_maps, core_ids=list(range(NCORES)))
    out = np.concatenate([np.asarray(r["out"]).reshape(NB, S, D) for r in res.results], axis=0)
    return out.astype(np.float32)


def make_in_maps(inputs):
    x = np.ascontiguousarray(np.asarray(inputs["x"], dtype=np.float32))
    mem = np.ascontiguousarray(np.asarray(inputs["mem"], dtype=np.float32))
    pos = np.ascontiguousarray(np.asarray(inputs["positions"], dtype=np.int32))
    maps = []
    for c in range(NCORES):
        m = {}
        m["x"] = x[c * NB:(c + 1) * NB].reshape(NB * S, D)
        m["mem"] = mem[c * NB:(c + 1) * NB].reshape(NB * 256, D)
        m["positions"] = pos[c * NB:(c + 1) * NB].reshape(NT, 128)
        for k, v in inputs.items():
            if k in ("x", "mem", "positions"):
                continue
            a = np.ascontiguousarray(np.asarray(v, dtype=np.float32))
            a = a[0]
            if a.ndim == 1:
                a = a.reshape(1, -1)
            m[k] = a
        maps.append(m)
    return maps
```

```python
import math
import numpy as np
import concourse.bass as bass
import concourse.mybir as mybir
from concourse.bass_utils import run_bass_kernel_spmd

F32 = mybir.dt.float32
BF16 = mybir.dt.bfloat16
I32 = mybir.dt.int32
ALU = mybir.AluOpType
AF = mybir.ActivationFunctionType
AX = mybir.AxisListType

NCORES = 8
D = 1024
S = 4096
NB = 2
NTS = S // 128
NT = NB * NTS
N_IN = 3688
EPS = 1e-6
NEG = -30000.0
EPOCH = 8192
NBIS = 16


class Res:
    __slots__ = ("name", "w", "rd", "sem", "cnt", "pend")

    def __init__(self, name):
        self.name = name
        self.w = None
        self.rd = []
        self.sem = None
        self.cnt = 0
        self.pend = None


class Op:
    __slots__ = ("eng", "idx", "fn", "waits", "signals", "dma", "semres", "val", "rank")

    def __init__(self, eng, idx, fn, dma, semres):
        self.eng = eng
        self.idx = idx
        self.fn = fn
        self.waits = []
        self.signals = False
        self.dma = dma
        self.semres = semres
        self.val = 0
        self.rank = -1


class T:
    def __init__(self, S_, name, shape, dtype, space="sb", kind=None):
        nc = S_.nc
        self.name = name
        if space == "sb":
            self.t = S_.stack.enter_context(nc.sbuf_tensor(name, list(shape), dtype))
        elif space == "ps":
            self.t = S_.stack.enter_context(nc.psum_tensor(name, list(shape), dtype))
        else:
            if kind is None:
                self.t = nc.dram_tensor(name, list(shape), dtype)
            else:
                self.t = nc.dram_tensor(name, list(shape), dtype, kind=kind)
        self.space = space
        self.res = Res(name)
        if space == "dr" and kind != "ExternalInput":
            self.res.pend = {}

    def __getitem__(self, k):
        if self.space == "dr":
            return self.t.ap()[k]
        return self.t[k]

    def ap(self):
        if self.space == "dr":
            return self.t.ap()
        return self.t[:]


ENGS = ("sp", "act", "dve", "pool", "pe")


class Sched:
    def __init__(self, nc, stack):
        self.nc = nc
        self.stack = stack
        self.semstack = stack
        self.ops = {e: [] for e in ENGS}
        self.seen = {e: {} for e in ENGS}
        self.semres_list = []

    @staticmethod
    def _res(x):
        return x.res if hasattr(x, 'res') else x

    def op(self, eng, meth, reads=(), writes=(), *args, dma=False, semres=None, **kw):
        lst = self.ops[eng]
        if dma:
            semres = self._res(semres)
        o = Op(eng, len(lst), (meth, args, kw), dma, semres)
        deps = []
        for r in reads:
            r = self._res(r)
            if r.pend is not None:
                for (sr, v) in r.pend.values():
                    fake = Op("sp", -1, None, True, sr)
                    fake.val = v
                    deps.append((fake, True))
                continue
            if r.w is not None:
                deps.append((r.w, True))
        for w in writes:
            w = self._res(w)
            if w.pend is not None:
                continue
            if w.w is not None and not (dma and w.w.dma):
                deps.append((w.w, False))
            for rd in w.rd:
                deps.append((rd, False))
        seen = self.seen[eng]
        dmax = {}
        for (p, raw) in deps:
            if p.dma:
                k_ = id(p.semres)
                if k_ not in dmax or dmax[k_].val < p.val:
                    dmax[k_] = p
        deps = [(p, raw) for (p, raw) in deps if not p.dma] + [(p, True) for p in dmax.values()]
        emax = {}
        for (p, raw) in deps:
            if not p.dma:
                if p.eng not in emax or emax[p.eng].idx < p.idx:
                    emax[p.eng] = p
        deps = [(p, raw) for (p, raw) in deps if p.dma] + [(p, True) for p in emax.values()]
        for (p, raw) in deps:
            if p is o:
                continue
            if p.dma:
                key = ("d", id(p.semres))
                if seen.get(key, 0) >= p.val:
                    continue
                seen[key] = p.val
                o.waits.append(p)
            else:
                if p.eng == eng:
                    if eng == "pe" and not dma:
                        continue
                key = ("e", p.eng)
                if seen.get(key, -1) >= p.idx:
                    continue
                seen[key] = p.idx
                p.signals = True
                o.waits.append(p)
        if dma:
            if semres.sem is None:
                semres.sem = self.semstack.enter_context(self.nc.semaphore("ds_" + semres.name))
                self.semres_list.append(semres)
            semres.cnt += 16
            o.val = semres.cnt
        for r in reads:
            r = self._res(r)
            if r.pend is None:
                r.rd.append(o)
        for w in writes:
            w = self._res(w)
            if w.pend is not None:
                assert dma
                w.pend[id(semres)] = (semres, o.val)
                continue
            w.w = o
            w.rd = []
        lst.append(o)
        return o

    def push(self):
        from contextlib import ExitStack
        self._saved = getattr(self, "_saved", [])
        self._saved.append(self.stack)
        self.stack = ExitStack()

    def pop(self):
        self.barrier()
        self.stack.close()
        self.stack = self._saved.pop()

    def barrier(self):
        last = {e: (self.ops[e][-1] if self.ops[e] else None) for e in ENGS}
        lastc = {}
        for e in ENGS:
            lc = None
            for o in reversed(self.ops[e]):
                if not o.dma and o.fn is not None:
                    lc = o
                    break
            lastc[e] = lc
        dmas = [(r, r.cnt) for r in self.semres_list]
        for e in ENGS:
            o = Op(e, len(self.ops[e]), None, False, None)
            seen = self.seen[e]
            for e2 in ENGS:
                p = lastc[e2]
                if p is None or e2 == e:
                    continue
                if seen.get(("e", e2), -1) >= p.idx:
                    continue
                seen[("e", e2)] = p.idx
                p.signals = True
                o.waits.append(p)
            for (r, cnt) in dmas:
                key = ("d", id(r))
                if seen.get(key, 0) >= cnt:
                    continue
                seen[key] = cnt
                fake = Op("sp", -1, None, True, r)
                fake.val = cnt
                o.waits.append(fake)
            self.ops[e].append(o)

    def dma(self, eng, out, in_, reads, writes, semres, **kw):
        return self.op(eng, "dma_start", reads, writes, dma=True, semres=semres, out=out, in_=in_, **kw)

    def finalize(self, final_wait_eng="sp"):
        nc = self.nc
        fin = Op(final_wait_eng, len(self.ops[final_wait_eng]), None, False, None)
        finwaits = [(r.sem, r.cnt) for r in self.semres_list]
        engsems = {}
        for e in ENGS:
            rank = 0
            for o in self.ops[e]:
                if o.signals and not o.dma:
                    o.rank = rank
                    rank += 1
            nep = (rank + EPOCH - 1) // EPOCH
            engsems[e] = [self.semstack.enter_context(nc.semaphore("es_%s_%d" % (e, i))) for i in range(nep)]
        self.engsems = engsems
        nsem = sum(len(v) for v in engsems.values()) + len(self.semres_list)
        self.nsem = nsem

        def replay(ename, e):
            for o in self.ops[ename]:
                for p in o.waits:
                    if p.dma:
                        e.wait_ge(p.semres.sem, p.val)
                    else:
                        e.wait_ge(engsems[p.eng][p.rank // EPOCH], p.rank % EPOCH + 1)
                if o.fn is None:
                    continue
                meth, args, kw = o.fn
                inst = getattr(e, meth)(*args, **kw)
                if o.dma:
                    inst.then_inc(o.semres.sem, 16)
                elif o.signals:
                    inst.then_inc(engsems[ename][o.rank // EPOCH], 1)
            if ename == final_wait_eng:
                for (sem, cnt) in finwaits:
                    e.wait_ge(sem, cnt)

        with nc.Block() as block:
            @block.sync
            def _(e):
                replay("sp", e)

            @block.scalar
            def _(e):
                replay("act", e)

            @block.vector
            def _(e):
                replay("dve", e)

            @block.gpsimd
            def _(e):
                replay("pool", e)

            @block.tensor
            def _(e):
                replay("pe", e)


def build(phases=(1, 2, 3, 4), debug=(), ntl=NT):
    from contextlib import ExitStack
    nc = bass.Bass("TRN2", target_bir_lowering=False)
    stack = ExitStack()
    S_ = Sched(nc, stack)
    op = S_.op

    def dram_in(name, shape, dtype=F32):
        return T(S_, name, shape, dtype, "dr", kind="ExternalInput")

    x_d = dram_in("x", [NB * S, D])
    mem_d = dram_in("mem", [NB * 256, D])
    pos_d = dram_in("positions", [NT, 128], I32)
    attn_g_d = dram_in("attn_norm_g", [1, D])
    w_in_d = dram_in("w_in", [D, N_IN])
    dsa_qg_d = dram_in("dsa_q_norm_g", [1, 64])
    dsa_kg_d = dram_in("dsa_k_norm_g", [1, 64])
    cq_g_d = dram_in("mla_cq_norm_g", [1, 256])
    ckv_g_d = dram_in("mla_ckv_norm_g", [1, 128])
    w_uq_d = dram_in("mla_w_uq", [256, 768])
    w_ukv_d = dram_in("mla_w_ukv", [128, 1024])
    mq_g_d = dram_in("mla_q_norm_g", [1, 96])
    mk_g_d = dram_in("mla_k_norm_g", [1, 96])
    w_bra_d = dram_in("w_branch_dsa", [512, D])
    w_brb_d = dram_in("w_branch_mla", [512, D])
    w_out_d = dram_in("w_out", [D, D])
    memx_g_d = dram_in("mem_x_norm_g", [1, D])
    mem_g_d = dram_in("mem_norm_g", [1, D])
    mem_wq_d = dram_in("mem_w_q", [D, 256])
    mem_wkv_d = dram_in("mem_w_kv", [D, 512])
    memq_g_d = dram_in("mem_q_norm_g", [1, 64])
    memk_g_d = dram_in("mem_k_norm_g", [1, 64])
    mem_wo_d = dram_in("mem_w_o", [256, D])
    moe_g_d = dram_in("moe_norm_g", [1, D])
    moe_wg_d = dram_in("moe_w_group", [D, 4])
    moe_we_d = dram_in("moe_w_expert", [D, 16])
    moe_b_d = dram_in("moe_expert_bias", [1, 16])
    moe_gate_d = dram_in("moe_w_gate", [16, D, 256])
    moe_up_d = dram_in("moe_w_up", [16, D, 256])
    moe_down_d = dram_in("moe_w_down", [16, 256, D])
    out_d = T(S_, "out", [NB * S, D], F32, "dr", kind="ExternalOutput")

    dbg_kind = lambda n: ("ExternalOutput" if (n in debug or n.startswith("dbg_")) else None)

    def scratch(name, shape, dtype):
        return T(S_, name, shape, dtype, "dr", kind=dbg_kind(name))

    qaT_d = scratch("qaT", [NT, 8, 64, 128], BF16)
    qiT_d = scratch("qiT", [NT, 8, 64, 128], BF16)
    kaT_d = scratch("kaT", [NB, 64, S], BF16)
    kiT_d = scratch("kiT", [NB, 64, S], BF16)
    va_d = scratch("va", [NB * S, 65], BF16)
    wi_d = scratch("wi", [NT, 128, 8], F32)
    qbT_d = scratch("qbT", [NB, 96, 8, S], BF16)
    kbT_d = scratch("kbT", [NB, 96, 8, S], BF16)
    vb_d = scratch("vb", [NB * S, 8 * 65], BF16)
    sg_d = scratch("sg", [NT, 128, 2048], BF16)
    oaT_d = scratch("oaT", [NT, 64, 8, 128], BF16)
    obT_d = scratch("obT", [NT, 64, 8, 128], BF16)
    x2_d = scratch("x2", [NB * S, D], F32)
    wgu_d = scratch("wgu", [16, 128, 8, 512], BF16)
    wgu_sem = Res("wgu_sem")
    if "dbg_negm" in debug:
        dbg_negm = scratch("dbg_negm", [ntl, 128, S], BF16)
        dbg_score = scratch("dbg_score", [ntl, 128, S], F32)

    def sb(name, shape, dtype=F32):
        return T(S_, name, shape, dtype, "sb")

    def ps(name, shape, dtype=F32):
        return T(S_, name, shape, dtype, "ps")

    io_i = sb("io_i", [128, 128], I32)
    io_f = sb("io_f", [128, 128])
    ident_f = sb("ident_f", [128, 128])
    ident_bf = sb("ident_bf", [128, 128], BF16)
    neghalf = sb("neghalf", [128, 16])
    ones_f = sb("ones_f", [128, 64])
    junk_act = sb("junk_act", [128, 1024], BF16)

    op("pool", "iota", [], [io_i], io_i[:, :], [[1, 128]], base=0, channel_multiplier=-1)
    op("dve", "tensor_copy", [io_i], [io_f], io_f[:, :], io_i[:, :])
    op("dve", "tensor_single_scalar", [io_f], [ident_f], ident_f[:, :], io_f[:, :], 0.0, ALU.is_equal)
    op("dve", "tensor_copy", [ident_f], [ident_bf], ident_bf[:, :], ident_f[:, :])
    op("dve", "memset", [], [neghalf], neghalf[:, :], -0.5)
    op("dve", "memset", [], [ones_f], ones_f[:, :], 1.0)

    def bcast_load(name, dt_, n):
        t = sb(name, [128, n])
        S_.dma("sp", t[:, :], dt_[0:1, :].broadcast_to([128, n]), [dt_], [t], t)
        return t

    def rstd_from_ss(ss, out, n, d, tmp):
        op("dve", "tensor_scalar", [ss], [tmp], tmp[:, 0:n], ss[:, 0:n], 1.0 / d, EPS, ALU.mult, ALU.add)
        op("pool", "tensor_tensor", [tmp, neghalf], [out], out[:, 0:n], tmp[:, 0:n], neghalf[:, 0:n], ALU.pow)

    def rms_heads(src3, src_res, dst3, dst_res, H, d, gbc, tmps):
        sq, ssq, r1, rstd, xn = tmps
        sq3 = sq[:, 0:H * d].rearrange("p (h d) -> p h d", h=H)
        xn3 = xn[:, 0:H * d].rearrange("p (h d) -> p h d", h=H)
        op("dve", "tensor_tensor", [src_res], [sq], sq3, src3, src3, ALU.mult)
        op("dve", "tensor_reduce", [sq], [ssq], ssq[:, 0:H], sq3, AX.X, ALU.add)
        rstd_from_ss(ssq, rstd, H, d, r1)
        op("dve", "tensor_tensor", [src_res, rstd], [xn], xn3, src3, rstd[:, 0:H].unsqueeze(2).broadcast_to([128, H, d]), ALU.mult)
        op("dve", "tensor_tensor", [xn, gbc], [dst_res], dst3, xn3, gbc[:, 0:d].unsqueeze(1).broadcast_to([128, H, d]), ALU.mult)

    def rope(src3, src_res, dst3, dst_res, H, d, cs, sn, trig_res, tmps, eng="pool"):
        t1, t2 = tmps
        hd = d // 2
        x1 = src3[:, :, 0:hd]
        x2 = src3[:, :, hd:d]
        csb = cs.unsqueeze(1).broadcast_to([128, H, hd])
        snb = sn.unsqueeze(1).broadcast_to([128, H, hd])
        a = t1[:, 0:H * hd].rearrange("p (h d) -> p h d", h=H)
        b = t2[:, 0:H * hd].rearrange("p (h d) -> p h d", h=H)
        op(eng, "tensor_tensor", [src_res, trig_res], [t1], a, x1, csb, ALU.mult)
        op(eng, "tensor_tensor", [src_res, trig_res], [t2], b, x2, snb, ALU.mult)
        op(eng, "tensor_tensor", [t1, t2], [dst_res], dst3[:, :, 0:hd], a, b, ALU.subtract)
        op(eng, "tensor_tensor", [src_res, trig_res], [t1], a, x2, csb, ALU.mult)
        op(eng, "tensor_tensor", [src_res, trig_res], [t2], b, x1, snb, ALU.mult)
        op(eng, "tensor_tensor", [t1, t2], [dst_res], dst3[:, :, hd:d], a, b, ALU.add)

    class View:
        def __init__(self, base_ap, res):
            self.base = base_ap
            self.res = res

        def __getitem__(self, k):
            return self.base[k]

    P2 = [ps("pz%d" % i, [128, 1024]) for i in range(4)]
    pbank = [View(P2[i // 2][:, (i % 2) * 512:(i % 2) * 512 + 512], Res("pb%d" % i)) for i in range(8)]

    TW = 48
    if 1 in phases:
        S_.push()
        sinT = sb("sinT", [128, NT * TW])
        cosT = sb("cosT", [128, NT * TW])
        S_.push()
        pos_i = sb("pos_i", [NT, 128], I32)
        pos_f = sb("pos_f", [NT, 128])
        posT = sb("posT", [128, NT])
        fr_i = sb("fr_i", [128, TW], I32)
        fr_f = sb("fr_f", [128, TW])
        freq = sb("freq", [128, TW])
        xs = sb("trig_xs", [128, NT * TW])
        xk_i = sb("trig_ki", [128, NT * TW], I32)
        xk_f = sb("trig_kf", [128, NT * TW])
        xa = sb("trig_a", [128, NT * TW])
        S_.dma("sp", pos_i[:, :], pos_d[:, :], [pos_d], [pos_i], pos_i)
        op("dve", "tensor_copy", [pos_i], [pos_f], pos_f[:, :], pos_i[:, :])
        op("pe", "transpose", [pos_f, ident_f], [pbank[0]], pbank[0][:, 0:NT], pos_f[:, :], ident_f[0:NT, 0:NT])
        op("dve", "tensor_copy", [pbank[0]], [posT], posT[:, :], pbank[0][:, 0:NT])
        op("pool", "iota", [], [fr_i], fr_i[:, :], [[1, TW]], base=0, channel_multiplier=0)
        op("dve", "tensor_copy", [fr_i], [fr_f], fr_f[:, :], fr_i[:, :])
        lt = math.log(10000.0)
        op("act", "activation", [fr_f], [freq], freq[:, 0:32], fr_f[:, 0:32], AF.Exp, scale=-lt / 32.0)
        op("act", "activation", [fr_f], [freq], freq[:, 32:48], fr_f[:, 32:48], AF.Exp, scale=-lt / 16.0, bias=2.0 * lt)
        xs3 = xs[:, :].rearrange("p (t w) -> p t w", w=TW)
        op("dve", "tensor_tensor", [posT, freq], [xs], xs3, posT[:, :].unsqueeze(2).broadcast_to([128, NT, TW]),
                                            freq[:, :].unsqueeze(1).broadcast_to([128, NT, TW]), ALU.mult)
        op("dve", "tensor_scalar", [xs], [xs], xs[:, :], xs[:, :], 1.0 / (2 * math.pi), None, ALU.mult)

        def trig(dst, shift):
            op("dve", "tensor_scalar", [xs], [xa], xa[:, :], xs[:, :], shift, None, ALU.add)
            op("dve", "tensor_copy", [xa], [xk_i], xk_i[:, :], xa[:, :])
            op("dve", "tensor_copy", [xk_i], [xk_f], xk_f[:, :], xk_i[:, :])
            op("dve", "tensor_tensor", [xa, xk_f], [xa], xa[:, :], xa[:, :], xk_f[:, :], ALU.subtract)
            op("dve", "tensor_single_scalar", [xa], [xk_f], xk_f[:, :], xa[:, :], 0.5, ALU.is_gt)
            op("dve", "tensor_tensor", [xa, xk_f], [xa], xa[:, :], xa[:, :], xk_f[:, :], ALU.subtract)
            op("dve", "tensor_single_scalar", [xa], [xk_f], xk_f[:, :], xa[:, :], -0.5, ALU.is_lt)
            op("dve", "tensor_tensor", [xa, xk_f], [xa], xa[:, :], xa[:, :], xk_f[:, :], ALU.add)
            op("act", "activation", [xa], [dst], dst[:, :], xa[:, :], AF.Sin, scale=2 * math.pi)

        trig(sinT, 0.0)
        trig(cosT, 0.25)
        S_.pop()

        sin3 = sinT[:, :].rearrange("p (t w) -> p t w", w=TW)
        cos3 = cosT[:, :].rearrange("p (t w) -> p t w", w=TW)

    if 1 in phases:
        g_attn = bcast_load("g_attn", attn_g_d, D)
        g_dq = bcast_load("g_dq", dsa_qg_d, 64)
        g_dk = bcast_load("g_dk", dsa_kg_d, 64)
        g_cq = bcast_load("g_cq", cq_g_d, 256)
        g_ckv = bcast_load("g_ckv", ckv_g_d, 128)
        g_mq = bcast_load("g_mq", mq_g_d, 96)
        g_mk = bcast_load("g_mk", mk_g_d, 96)
        win = sb("win", [128, 8 * N_IN], BF16)
        win3 = win[:, :].rearrange("p (k n) -> p k n", k=8)
        for kc in range(8):
            S_.dma("pool", win3[:, kc, :], w_in_d[kc * 128:(kc + 1) * 128, :], [w_in_d], [win], win)
        wuq = sb("wuq", [128, 2 * 768], BF16)
        wuq3 = wuq[:, :].rearrange("p (k n) -> p k n", k=2)
        for kc in range(2):
            S_.dma("pool", wuq3[:, kc, :], w_uq_d[kc * 128:(kc + 1) * 128, :], [w_uq_d], [wuq], wuq)
        wukv = sb("wukv", [128, 1024], BF16)
        S_.dma("pool", wukv[:, :], w_ukv_d[:, :], [w_ukv_d], [wukv], wukv)
        if 4 in phases:
            for e in range(16):
                S_.dma("pool", wgu_d[e][:, :, 0:256], moe_gate_d[e].rearrange("(k p) f -> p k f", p=128), [moe_gate_d], [wgu_d], wgu_sem)
                S_.dma("pool", wgu_d[e][:, :, 256:512], moe_up_d[e].rearrange("(k p) f -> p k f", p=128), [moe_up_d], [wgu_d], wgu_sem)


        xin = [sb("xin%d" % i, [128, D]) for i in range(2)]
        ss = sb("ss", [128, 16])
        r1 = sb("r1", [128, 16])
        rstd = sb("rstd", [128, 16])
        h_bf = sb("h_bf", [128, D], BF16)
        hT = sb("hT", [128, D], BF16)
        proj2 = [sb("proj%d" % i, [128, N_IN]) for i in range(2)]
        t_sq = sb("t_sq", [128, 1024])
        t_xn = sb("t_xn", [128, 1024])
        t_ssq = sb("t_ssq", [128, 16])
        ssqA = sb("ssqA", [128, 16])
        eps_t = sb("eps_t", [128, 1])
        op("dve", "memset", [], [eps_t], eps_t[:, :], EPS)
        r1A = sb("r1A", [128, 16])
        rstdA = sb("rstdA", [128, 16])
        ssqB = sb("ssqB", [128, 16])
        r1B = sb("r1B", [128, 16])
        rstdB = sb("rstdB", [128, 16])
        invdA = sb("invdA", [128, 16])
        invdB = sb("invdB", [128, 16])
        op("dve", "memset", [], [invdA], invdA[:, :], 1.0 / 64)
        op("dve", "memset", [], [invdA], invdA[:, 9:10], 1.0 / 256)
        op("dve", "memset", [], [invdA], invdA[:, 10:11], 1.0 / 128)
        op("dve", "memset", [], [invdB], invdB[:, :], 1.0 / 96)
        t_r1 = sb("t_r1", [128, 16])
        t_rstd = sb("t_rstd", [128, 16])
        tmps = (t_sq, t_ssq, t_r1, t_rstd, t_xn)
        t_a = sb("t_a", [128, 512])
        t_b = sb("t_b", [128, 512])
        t_c = sb("t_c", [128, 256])
        t_d = sb("t_d", [128, 256])
        qan = sb("qan", [128, 512])
        qa_sb = sb("qa_sb", [128, 512], BF16)
        qi_sb = sb("qi_sb", [128, 512], BF16)
        kk_n = sb("kk_n", [128, 64])
        kk_sb = sb("kk_sb", [128, 128], BF16)
        cqn = sb("cqn", [128, 256], BF16)
        cqT = sb("cqT", [128, 256], BF16)
        ckvn = sb("ckvn", [128, 128], BF16)
        ckvT = sb("ckvT", [128, 128], BF16)
        qb_f = sb("qb_f", [128, 768])
        qb_n = sb("qb_n", [128, 768])
        qb_sb = sb("qb_sb", [128, 768], BF16)
        kv_f = sb("kv_f", [128, 1024])
        kb_f = sb("kb_f", [128, 768])
        kb_n = sb("kb_n", [128, 768])
        kb_sb = sb("kb_sb", [128, 768], BF16)
        qaT_s = [sb("qaT_s%d" % i, [128, 512], BF16) for i in range(2)]
        qiT_s = [sb("qiT_s%d" % i, [128, 512], BF16) for i in range(2)]
        kkT_s = [sb("kkT_s%d" % i, [128, 128], BF16) for i in range(2)]
        va_s = [sb("va_s%d" % i, [128, 65], BF16) for i in range(2)]
        wi_s = [sb("wi_s%d" % i, [128, 8]) for i in range(2)]
        qbT_s = [sb("qbT_s%d" % i, [128, 1024], BF16) for i in range(2)]
        kbT_s = [sb("kbT_s%d" % i, [128, 1024], BF16) for i in range(2)]
        vb_s = [sb("vb_s%d" % i, [128, 8 * 65], BF16) for i in range(2)]
        sg_s = [sb("sg_s%d" % i, [128, 2048], BF16) for i in range(2)]
        for i in range(2):
            op("dve", "memset", [], [va_s[i]], va_s[i][:, 64:65], 1.0)
            op("dve", "memset", [], [vb_s[i]], vb_s[i][:, :], 1.0)

        def pbf(i):
            return pbank[i][:, :].bitcast(BF16)

        OFF = [0, 512, 576, 640, 1152, 1216, 1224, 1480, 1608, 1640, 2664, 3688]
        evac_flip = [0]

        def evac(dst_ap, dst_res, src_ap, src_res):
            if evac_flip[0] % 2 == 0:
                op("act", "copy", [src_res], [dst_res], dst_ap, src_ap)
            else:
                op("dve", "tensor_copy", [src_res], [dst_res], dst_ap, src_ap)
            evac_flip[0] += 1

        S_.dma("sp", xin[0][:, :], x_d[0:128, :], [x_d], [xin[0]], xin[0])
        def stageA(ti):
            proj = proj2[ti % 2]
            b, t = divmod(ti, NTS)
            sl = ti % 2
            xt = xin[sl]
            if ti + 1 < ntl:
                S_.dma("sp", xin[1 - sl][:, :], x_d[(ti + 1) * 128:(ti + 2) * 128, :], [x_d], [xin[1 - sl]], xin[1 - sl])
            op("act", "activation", [xt], [ss], junk_act[:, :], xt[:, :], AF.Square, accum_out=ss[:, 0:1])
            op("act", "activation", [ss, eps_t], [r1], r1[:, 0:1], ss[:, 0:1], AF.Sqrt, scale=1.0 / D, bias=eps_t[:, 0:1])
            op("dve", "reciprocal", [r1], [rstd], rstd[:, 0:1], r1[:, 0:1])
            op("dve", "scalar_tensor_tensor", [xt, rstd, g_attn], [h_bf], h_bf[:, :], xt[:, :], rstd[:, 0:1], g_attn[:, :], ALU.mult, ALU.mult)
            yield
            tpb = 4 + (ti % 2)
            for kc in range(8):
                op("pe", "transpose", [h_bf, ident_bf], [pbank[tpb]], pbf(tpb)[:, kc * 128:(kc + 1) * 128], h_bf[:, kc * 128:(kc + 1) * 128], ident_bf[:, :])
            op("act", "copy", [pbank[tpb]], [hT], hT[:, :], pbf(tpb)[:, :])
            yield
            for nchunk in range(8):
                c0 = nchunk * 512
                w = min(512, N_IN - c0)
                pb = pbank[nchunk % 2]
                for kc in range(8):
                    op("pe", "matmul", [hT, win], [pb], pb[:, 0:w], hT[:, kc * 128:(kc + 1) * 128], win3[:, kc, c0:c0 + w],
                                                                          start=(kc == 0), stop=(kc == 7))
                op("act", "copy", [pb], [proj], proj[:, c0:c0 + w], pb[:, 0:w])
                yield

        def stageB(ti):
            proj = proj2[ti % 2]
            b, t = divmod(ti, NTS)
            sl = ti % 2
            cs64 = cos3[:, ti, 0:32]
            sn64 = sin3[:, ti, 0:32]
            cs32 = cos3[:, ti, 32:48]
            sn32 = sin3[:, ti, 32:48]
            trig_res = sinT
            dq3 = proj[:, 0:512].rearrange("p (h d) -> p h d", h=8)
            dk3 = proj[:, 512:576].rearrange("p (h d) -> p h d", h=1)
            cq3 = proj[:, 1224:1480].rearrange("p (h d) -> p h d", h=1)
            ckv3 = proj[:, 1480:1608].rearrange("p (h d) -> p h d", h=1)
            qan3 = qan[:, :].rearrange("p (h d) -> p h d", h=8)
            kkn3 = kk_n[:, :].rearrange("p (h d) -> p h d", h=1)

            def ss_part(src3, src_res, H, d, ssq, col):
                sq3 = t_sq[:, 0:H * d].rearrange("p (h d) -> p h d", h=H)
                op("dve", "tensor_tensor", [src_res], [t_sq], sq3, src3, src3, ALU.mult)
                op("dve", "tensor_reduce", [t_sq], [ssq], ssq[:, col:col + H], sq3, AX.X, ALU.add)

            def rstd_all(ssq, invd, r1_, rstd_, n):
                op("dve", "tensor_tensor", [ssq, invd], [r1_], r1_[:, 0:n], ssq[:, 0:n], invd[:, 0:n], ALU.mult)
                op("act", "activation", [r1_, eps_t], [r1_], r1_[:, 0:n], r1_[:, 0:n], AF.Sqrt, bias=eps_t[:, 0:1])
                op("dve", "reciprocal", [r1_], [rstd_], rstd_[:, 0:n], r1_[:, 0:n])

            def apply_part(src3, src_res, dst3, dst_res, H, d, rstd_, col, gbc):
                xn3 = t_xn[:, 0:H * d].rearrange("p (h d) -> p h d", h=H)
                op("dve", "tensor_tensor", [src_res, rstd_], [t_xn], xn3, src3, rstd_[:, col:col + H].unsqueeze(2).broadcast_to([128, H, d]), ALU.mult)
                op("dve", "tensor_tensor", [t_xn, gbc], [dst_res], dst3, xn3, gbc[:, 0:d].unsqueeze(1).broadcast_to([128, H, d]), ALU.mult)

            ss_part(cq3, proj, 1, 256, ssqA, 9)
            ss_part(ckv3, proj, 1, 128, ssqA, 10)
            ss_part(dq3, proj, 8, 64, ssqA, 0)
            ss_part(dk3, proj, 1, 64, ssqA, 8)
            rstd_all(ssqA, invdA, r1A, rstdA, 11)
            yield
            rope(proj[:, 640:1152].rearrange("p (h d) -> p h d", h=8), proj, qi_sb[:, :].rearrange("p (h d) -> p h d", h=8), qi_sb,
                 8, 64, cs64, sn64, trig_res, (t_a, t_b))
            rope(proj[:, 1152:1216].rearrange("p (h d) -> p h d", h=1), proj, kk_sb[:, 64:128].rearrange("p (h d) -> p h d", h=1), kk_sb,
                 1, 64, cs64, sn64, trig_res, (t_a, t_b))
            op("pool", "tensor_copy", [proj], [va_s[sl]], va_s[sl][:, 0:64], proj[:, 576:640])
            op("pool", "tensor_copy", [proj], [wi_s[sl]], wi_s[sl][:, :], proj[:, 1216:1224])
            yield
            apply_part(cq3, proj, cqn[:, :].rearrange("p (h d) -> p h d", h=1), cqn, 1, 256, rstdA, 9, g_cq)
            apply_part(ckv3, proj, ckvn[:, :].rearrange("p (h d) -> p h d", h=1), ckvn, 1, 128, rstdA, 10, g_ckv)
            yield
            tq = 6
            for kc in range(2):
                op("pe", "transpose", [cqn, ident_bf], [pbank[tq]], pbf(tq)[:, kc * 128:(kc + 1) * 128], cqn[:, kc * 128:(kc + 1) * 128], ident_bf[:, :])
            op("pe", "transpose", [ckvn, ident_bf], [pbank[tq]], pbf(tq)[:, 256:384], ckvn[:, :], ident_bf[:, :])
            op("act", "copy", [pbank[tq]], [cqT, ckvT], cqT[:, :], pbf(tq)[:, 0:256])
            op("act", "copy", [pbank[tq]], [ckvT], ckvT[:, :], pbf(tq)[:, 256:384])
            yield
            for (c0, w, pbi) in ((0, 512, 2), (512, 256, 3)):
                pb = pbank[pbi]
                for kc in range(2):
                    op("pe", "matmul", [cqT, wuq], [pb], pb[:, 0:w], cqT[:, kc * 128:(kc + 1) * 128], wuq3[:, kc, c0:c0 + w],
                       start=(kc == 0), stop=(kc == 1))
                op("act", "copy", [pb], [qb_f], qb_f[:, c0:c0 + w], pb[:, 0:w])
            for (c0, pbi) in ((0, 2), (512, 3)):
                pb = pbank[pbi]
                op("pe", "matmul", [ckvT, wukv], [pb], pb[:, 0:512], ckvT[:, :], wukv[:, c0:c0 + 512], start=True, stop=True)
                op("act", "copy", [pb], [kv_f], kv_f[:, c0:c0 + 512], pb[:, 0:512])
            apply_part(dq3, proj, qan3, qan, 8, 64, rstdA, 0, g_dq)
            yield
            apply_part(dk3, proj, kkn3, kk_n, 1, 64, rstdA, 8, g_dk)
            yield
            rope(qan3, qan, qa_sb[:, :].rearrange("p (h d) -> p h d", h=8), qa_sb, 8, 64, cs64, sn64, trig_res, (t_c, t_d), eng="dve")
            rope(kkn3, kk_n, kk_sb[:, 0:64].rearrange("p (h d) -> p h d", h=1), kk_sb, 1, 64, cs64, sn64, trig_res, (t_c, t_d), eng="dve")
            yield
            qbf3 = qb_f[:, :].rearrange("p (h d) -> p h d", h=8)
            qbn3 = qb_n[:, :].rearrange("p (h d) -> p h d", h=8)
            qbs3 = qb_sb[:, :].rearrange("p (h d) -> p h d", h=8)
            kv3 = kv_f[:, :].rearrange("p (h d) -> p h d", h=8)
            kbf3 = kb_f[:, :].rearrange("p (h d) -> p h d", h=8)
            kbn3 = kb_n[:, :].rearrange("p (h d) -> p h d", h=8)
            kbs3 = kb_sb[:, :].rearrange("p (h d) -> p h d", h=8)
            vbs3 = vb_s[sl][:, :].rearrange("p (h d) -> p h d", h=8)
            op("act", "copy", [kv_f], [kb_f], kbf3[:, :, 0:64], kv3[:, :, 0:64])
            op("pool", "tensor_copy", [proj], [kb_f], kbf3[:, :, 64:96], proj[:, 1608:1640].unsqueeze(1).broadcast_to([128, 8, 32]))
            op("act", "copy", [kv_f], [vb_s[sl]], vbs3[:, :, 0:64], kv3[:, :, 64:128])
            ss_part(qbf3, qb_f, 8, 96, ssqB, 0)
            ss_part(kbf3, kb_f, 8, 96, ssqB, 8)
            rstd_all(ssqB, invdB, r1B, rstdB, 16)
            yield
            apply_part(qbf3, qb_f, qbn3, qb_n, 8, 96, rstdB, 0, g_mq)
            apply_part(kbf3, kb_f, kbn3, kb_n, 8, 96, rstdB, 8, g_mk)
            yield
            op("act", "copy", [qb_n], [qb_sb], qbs3[:, :, 0:64], qbn3[:, :, 0:64])
            rope(qbn3[:, :, 64:96], qb_n, qbs3[:, :, 64:96], qb_sb, 8, 32, cs32, sn32, trig_res, (t_a, t_b))
            op("act", "copy", [kb_n], [kb_sb], kbs3[:, :, 0:64], kbn3[:, :, 0:64])
            rope(kbn3[:, :, 64:96], kb_n, kbs3[:, :, 64:96], kb_sb, 8, 32, cs32, sn32, trig_res, (t_a, t_b))
            yield
            op("act", "activation", [proj], [sg_s[sl]], sg_s[sl][:, :], proj[:, 1640:3688], AF.Sigmoid)
            tr = 7
            for j in range(4):
                op("pe", "transpose", [qa_sb, ident_bf], [pbank[tr]], pbf(tr)[:, j * 128:(j + 1) * 128], qa_sb[:, j * 128:(j + 1) * 128], ident_bf[:, :])
            for j in range(4):
                op("pe", "transpose", [qi_sb, ident_bf], [pbank[tr]], pbf(tr)[:, 512 + j * 128:512 + (j + 1) * 128], qi_sb[:, j * 128:(j + 1) * 128], ident_bf[:, :])
            op("act", "copy", [pbank[tr]], [qaT_s[sl]], qaT_s[sl][:, :], pbf(tr)[:, 0:512])
            op("dve", "tensor_copy", [pbank[tr]], [qiT_s[sl]], qiT_s[sl][:, :], pbf(tr)[:, 512:1024])
            yield
            op("pe", "transpose", [kk_sb, ident_bf], [pbank[tq]], pbf(tq)[:, 384:512], kk_sb[:, :], ident_bf[:, :])
            op("act", "copy", [pbank[tq]], [kkT_s[sl]], kkT_s[sl][:, :], pbf(tq)[:, 384:512])
            for h in range(8):
                op("pe", "transpose", [qb_sb, ident_bf], [pbank[tr]], pbf(tr)[0:96, h * 128:(h + 1) * 128], qb_sb[:, h * 96:(h + 1) * 96], ident_bf[:, :])
            op("act", "copy", [pbank[tr]], [qbT_s[sl]], qbT_s[sl][0:96, :], pbf(tr)[0:96, :])
            yield
            for h in range(8):
                op("pe", "transpose", [kb_sb, ident_bf], [pbank[tr]], pbf(tr)[0:96, h * 128:(h + 1) * 128], kb_sb[:, h * 96:(h + 1) * 96], ident_bf[:, :])
            op("dve", "tensor_copy", [pbank[tr]], [kbT_s[sl]], kbT_s[sl][0:96, :], pbf(tr)[0:96, :])
            tok0 = ti * 128
            S_.dma("sp", qaT_d[ti].rearrange("(j hh) d q -> (hh d) j q", hh=2), qaT_s[sl][:, :].rearrange("p (j q) -> p j q", j=4),
                   [qaT_s[sl]], [qaT_d], qaT_s[sl])
            S_.dma("sp", qiT_d[ti].rearrange("(j hh) d q -> (hh d) j q", hh=2), qiT_s[sl][:, :].rearrange("p (j q) -> p j q", j=4),
                   [qiT_s[sl]], [qiT_d], qiT_s[sl])
            S_.dma("sp", kaT_d[b, :, t * 128:(t + 1) * 128], kkT_s[sl][0:64, :], [kkT_s[sl]], [kaT_d], kkT_s[sl])
            S_.dma("sp", kiT_d[b, :, t * 128:(t + 1) * 128], kkT_s[sl][64:128, :], [kkT_s[sl]], [kiT_d], kkT_s[sl])
            S_.dma("sp", va_d[tok0:tok0 + 128, :], va_s[sl][:, :], [va_s[sl]], [va_d], va_s[sl])
            S_.dma("sp", wi_d[ti], wi_s[sl][:, :], [wi_s[sl]], [wi_d], wi_s[sl])
            S_.dma("sp", qbT_d[b, :, :, t * 128:(t + 1) * 128], qbT_s[sl][0:96, :].rearrange("p (h q) -> p h q", h=8),
                   [qbT_s[sl]], [qbT_d], qbT_s[sl])
            S_.dma("sp", kbT_d[b, :, :, t * 128:(t + 1) * 128], kbT_s[sl][0:96, :].rearrange("p (h q) -> p h q", h=8),
                   [kbT_s[sl]], [kbT_d], kbT_s[sl])
            S_.dma("sp", vb_d[tok0:tok0 + 128, :], vb_s[sl][:, :], [vb_s[sl]], [vb_d], vb_s[sl])
            S_.dma("sp", sg_d[ti], sg_s[sl][:, :], [sg_s[sl]], [sg_d], sg_s[sl])


        def interleave1(gens):
            alive = list(gens)
            while alive:
                for g_ in list(alive):
                    try:
                        next(g_)
                    except StopIteration:
                        alive.remove(g_)

        interleave1([stageA(0)])
        for ti in range(ntl):
            gs_ = [stageB(ti)]
            if ti + 1 < ntl:
                gs_.insert(0, stageA(ti + 1))
            interleave1(gs_)
        S_.pop()

    if 2 in phases:
        phase2(S_, locals())
    if 3 in phases:
        phase3(S_, locals())
    if 4 in phases:
        phase4(S_, locals())
    S_.finalize()
    return nc, stack


class Env:
    def __init__(self, d):
        self.__dict__.update(d)


def phase2(S_, envd):
    E = Env(envd)
    op = S_.op
    sb, pbank, P2 = E.sb, E.pbank, E.P2
    ident_f, ident_bf, io_f, ones_f = E.ident_f, E.ident_bf, E.io_f, E.ones_f
    ntl = E.ntl
    S_.push()
    ident8 = sb("ident8", [128, 1024], BF16)
    op("dve", "tensor_copy", [ident_bf], [ident8], ident8[:, :].rearrange("p (r q) -> p r q", r=8),
       ident_bf[:, :].unsqueeze(1).broadcast_to([128, 8, 128]))
    caus_f = sb("caus_f", [128, 128])
    op("dve", "tensor_scalar", [io_f], [caus_f], caus_f[:, :], io_f[:, :], 0.0, NEG, ALU.is_gt, ALU.mult)
    maskT = [sb("maskT%d" % j, [128, 512], BF16) for j in range(4)]
    pow2 = sb("pow2", [128, NBIS + 1])
    S_.push()
    mk_i = sb("mk_i", [128, 512], I32)
    mk_f = sb("mk_f", [128, 512])
    op("pool", "iota", [], [mk_i], mk_i[:, :], [[1, 512]], base=0, channel_multiplier=-1)
    op("dve", "tensor_copy", [mk_i], [mk_f], mk_f[:, :], mk_i[:, :])
    for j in range(4):
        op("dve", "tensor_scalar", [mk_f], [maskT[j]], maskT[j][:, :], mk_f[:, :], float(128 * j), NEG, ALU.is_lt, ALU.mult)
    p2_i = sb("p2_i", [128, NBIS + 1], I32)
    p2_f = sb("p2_f", [128, NBIS + 1])
    op("pool", "iota", [], [p2_i], p2_i[:, :], [[1, NBIS + 1]], base=0, channel_multiplier=0)
    op("dve", "tensor_copy", [p2_i], [p2_f], p2_f[:, :], p2_i[:, :])
    op("act", "activation", [p2_f], [pow2], pow2[:, :], p2_f[:, :], AF.Exp, scale=-math.log(2.0))
    S_.pop()

    kk_sb = sb("kk2_sb", [128, S], BF16)
    va_sb = sb("va_sb", [128, NTS * 65], BF16)
    kbT_sb = sb("kbT_sb", [128, 8 * S], BF16)
    op("dve", "memset", [], [kbT_sb], kbT_sb[96:128, :], 0.0)
    vb_sb = sb("vb_sb", [128, NTS * 8 * 65], BF16)
    va3 = va_sb[:, :].rearrange("p (k c) -> p k c", c=65)
    kbT3 = kbT_sb[:, :].rearrange("p (h s) -> p h s", h=8)
    vb4 = vb_sb[:, :].rearrange("p (k h c) -> p k h c", h=8, c=65)
    qa_p = [sb("qa_p%d" % i, [128, 1024], BF16) for i in range(2)]
    qi_p = [sb("qi_p%d" % i, [128, 1024], BF16) for i in range(2)]
    for i in range(2):
        op("dve", "memset", [], [qa_p[i]], qa_p[i][64:128, :], 0.0)
        op("dve", "memset", [], [qi_p[i]], qi_p[i][0:64, :], 0.0)
    wi_t = [sb("wi_t%d" % i, [128, 8]) for i in range(2)]
    Dh2 = [sb("Dh0", [128, 1024], BF16)] * 2
    score2 = [sb("score%d" % i, [128, S]) for i in range(2)]
    junk = sb("junk_dve", [128, S], mybir.dt.uint8)
    negm2 = [sb("negm%d" % i, [128, S], BF16) for i in range(2)]
    amax = sb("amax", [128, 1])
    s0 = sb("s0", [128, 1])
    hw = sb("hw", [128, NBIS + 1])
    mid = sb("mid", [128, 1])
    cnt = sb("cnt", [128, 1])
    dd = sb("dd", [128, 1])
    lo = sb("lo", [128, 1])
    NPT = 4
    PT = [sb("PT%d" % i, [128, 1024], BF16) for i in range(NPT)]
    oaT_s = [sb("oaT_s%d" % i, [64, 1024], BF16) for i in range(2)]
    qbT_h = [sb("qbT_h%d" % i, [128, 512], BF16) for i in range(3)]
    for i in range(3):
        op("dve", "memset", [], [qbT_h[i]], qbT_h[i][96:128, :], 0.0)
    obT_h = [sb("obT_h%d" % i, [64, 512], BF16) for i in range(2)]
    qhrot = [0]
    ohrot = [0]
    zrot = [0]
    ptrot = [0]
    rhrot = [0]

    deferred = []

    def flush():
        while deferred:
            deferred.pop(0)()

    oaU = [sb("oaU%d" % i, [64, 1024], BF16) for i in range(2)]
    lnr = [sb("lnr0", [65, 512])] * 2
    r_bf = [sb("r_bf0", [65, 1024], BF16)] * 2
    ones_bf = sb("ones_bf", [65, 64], BF16)
    op("dve", "memset", [], [ones_bf], ones_bf[:, :], 1.0)
    oarot = [0]

    def normalize(oacc_ap, oacc_res, width, dst_ap, dst_res, after=None):
        flush()
        i_ = oarot[0] % 2
        oarot[0] += 1
        ou, ln_, rb = oaU[i_], lnr[i_], r_bf[i_]
        for c in range(width // 512):
            op("act", "activation", oacc_res, [ln_], ln_[64:65, 0:512], oacc_ap[64:65, c * 512:(c + 1) * 512], AF.Ln)
            op("act", "activation", [ln_], [rb], rb[64:65, c * 512:(c + 1) * 512], ln_[64:65, 0:512], AF.Exp, scale=-1.0)
        op("act", "copy", oacc_res, [ou], ou[:, 0:width], oacc_ap[0:64, :])

        def partB():
            bcr = [pbank[6], pbank[7]]
            for c in range(width // 512):
                op("pe", "matmul", [ones_bf, rb], [bcr[c]], bcr[c][0:64, 0:512], ones_bf[64:65, 0:64], rb[64:65, c * 512:(c + 1) * 512],
                   start=True, stop=True)
            op("dve", "tensor_tensor", [ou] + bcr[0:width // 512], [dst_res], dst_ap, ou[:, 0:width], P2[3][0:64, 0:width], ALU.mult)
            if after is not None:
                after()
        deferred.append(partB)

    ntile_seq = [min(NTS, max(0, ntl - b * NTS)) for b in range(NB)]
    for b in range(NB):
        nts = ntile_seq[b]
        if nts == 0:
            continue
        nkeys = nts * 128
        S_.dma("sp", kk_sb[0:64, 0:nkeys], E.kaT_d[b, :, 0:nkeys], [E.kaT_d], [kk_sb], kk_sb)
        S_.dma("sp", kk_sb[64:128, 0:nkeys], E.kiT_d[b, :, 0:nkeys], [E.kiT_d], [kk_sb], kk_sb)
        S_.dma("sp", va3[:, 0:nts, :], E.va_d[b * S:b * S + nkeys, :].rearrange("(k p) c -> p k c", p=128), [E.va_d], [va_sb], va_sb)
        for h in range(8):
            S_.dma("sp", kbT3[0:96, h, 0:nkeys], E.kbT_d[b, :, h, 0:nkeys], [E.kbT_d], [kbT_sb], kbT_sb)
        S_.dma("sp", vb_sb[:, 0:nts * 520].rearrange("p (k c) -> p k c", c=520),
               E.vb_d[b * S:b * S + nkeys, :].rearrange("(k p) c -> p k c", p=128), [E.vb_d], [vb_sb], vb_sb)

        def qa_load(t):
            ti = b * NTS + t
            sl = t % 2
            S_.dma("sp", qa_p[sl][0:64, :].rearrange("p (h q) -> p h q", h=8), E.qaT_d[ti].rearrange("h d q -> d h q"), [E.qaT_d], [qa_p[sl]], qa_p[sl])

        def dsa_index(t):
            ti = b * NTS + t
            sl = t % 2
            score = score2[t % 2]
            nk = 128 * (t + 1)
            S_.dma("sp", qi_p[sl][64:128, :].rearrange("p (h q) -> p h q", h=8), E.qiT_d[ti].rearrange("h d q -> d h q"), [E.qiT_d], [qi_p[sl]], qi_p[sl])
            S_.dma("sp", wi_t[sl][:, :], E.wi_d[ti], [E.wi_d], [wi_t[sl]], wi_t[sl])
            Dh = Dh2[t % 2]
            op("pool", "tensor_tensor", [ident_f, wi_t[sl]], [Dh], Dh[:, :].rearrange("p (h q) -> p h q", h=8),
               ident_f[:, :].unsqueeze(1).broadcast_to([128, 8, 128]), wi_t[sl][:, :].unsqueeze(2).broadcast_to([128, 8, 128]), ALU.mult)
            nch = (nk + 511) // 512
            items = [(c, j) for c in range(nch) for j in range(4)]

            def zmm(c, j):
                w = min(512, nk - 512 * c)
                zi = zrot[0] % 2
                zrot[0] += 1
                z = P2[zi]
                zres = [pbank[2 * zi], pbank[2 * zi + 1]]
                for a_ in range(2):
                    h = 2 * j + a_
                    op("pe", "matmul", [qi_p[sl], kk_sb], [zres[a_]], z[:, a_ * 512:a_ * 512 + w], qi_p[sl][:, h * 128:(h + 1) * 128],
                       kk_sb[:, 512 * c:512 * c + w], start=True, stop=True)
                return z, zres

            znext = zmm(*items[0])
            for i, (c, j) in enumerate(items):
                w = min(512, nk - 512 * c)
                z, zres = znext
                if i + 1 < len(items):
                    znext = zmm(*items[i + 1])
                sc = pbank[6 + (c % 2)]
                rh = PT[ptrot[0] % NPT]
                ptrot[0] += 1
                z3 = z[:, :].rearrange("p (a n) -> p a n", a=2)[:, :, 0:w]
                r3 = rh[:, :].rearrange("p (a n) -> p a n", a=2)[:, :, 0:w]
                op("act", "activation", zres, [rh], r3, z3, AF.Relu)
                for a_ in range(2):
                    h = 2 * j + a_
                    op("pe", "matmul", [Dh, rh], [sc], sc[:, 0:w], Dh[:, h * 128:(h + 1) * 128], rh[:, a_ * 512:a_ * 512 + w],
                       start=(h == 0), stop=(h == 7))
                if j == 3:
                    op("act", "copy", [sc], [score], score[:, 512 * c:512 * c + w], sc[:, 0:w])
                if j == 3 and c == 0:
                    flush()

        def dsa_bisect(t):
            nk = 128 * (t + 1)
            score = score2[t % 2]
            negm = negm2[t % 2]
            op("dve", "tensor_reduce", [score], [amax], amax[:, :], score[:, 0:nk], AX.X, ALU.max, apply_absolute_value=True)
            op("dve", "tensor_tensor", [score, caus_f], [score], score[:, nk - 128:nk], score[:, nk - 128:nk], caus_f[:, :], ALU.add)
            op("dve", "tensor_scalar", [amax], [s0], s0[:, :], amax[:, :], 1.001, 1e-6, ALU.mult, ALU.add)
            op("dve", "tensor_scalar", [pow2, s0], [hw], hw[:, :], pow2[:, :], s0[:, 0:1], None, ALU.mult)
            op("dve", "memset", [], [mid], mid[:, :], 0.0)
            for k in range(NBIS):
                op("dve", "tensor_scalar", [score, mid], [junk, cnt], junk[:, 0:nk], score[:, 0:nk], mid[:, 0:1], 0.0, ALU.is_ge, ALU.add,
                   accum_out=cnt[:, 0:1])
                op("dve", "tensor_scalar", [cnt, hw], [dd], dd[:, :], cnt[:, :], 255.5, hw[:, k:k + 1], ALU.is_ge, ALU.mult)
                op("dve", "scalar_tensor_tensor", [dd, hw, mid], [mid], mid[:, :], dd[:, :], hw[:, k + 1:k + 2], mid[:, :], ALU.subtract, ALU.add)
            op("dve", "tensor_scalar", [mid, hw], [lo], lo[:, :], mid[:, :], hw[:, NBIS:NBIS + 1], None, ALU.subtract)
            op("dve", "tensor_scalar", [score, lo], [negm], negm[:, 0:nk], score[:, 0:nk], lo[:, 0:1], NEG, ALU.is_lt, ALU.mult)
            if "dbg_negm" in E.debug:
                ti = b * NTS + t
                S_.dma("pool", E.dbg_negm[ti, :, 0:nk], negm[:, 0:nk], [negm], [E.dbg_negm], negm)
                S_.dma("pool", E.dbg_score[ti, :, 0:nk], score[:, 0:nk], [score], [E.dbg_score], score)

        def dsa_attn(t):
            ti = b * NTS + t
            sl = t % 2
            negm = negm2[t % 2]
            oacc = P2[2]
            oacc_res = [pbank[4], pbank[5]]

            def qk(kb):
                zi = (zrot[0] % 2)
                zrot[0] += 1
                z = P2[zi]
                zres = [pbank[2 * zi], pbank[2 * zi + 1]]
                for c in range(2):
                    op("pe", "matmul", [kk_sb, qa_p[sl]], [zres[c]], z[:, c * 512:(c + 1) * 512], kk_sb[:, kb * 128:(kb + 1) * 128],
                       qa_p[sl][:, c * 512:(c + 1) * 512], start=True, stop=False)
                    op("pe", "matmul", [negm, ident8], [zres[c]], z[:, c * 512:(c + 1) * 512], negm[:, kb * 128:(kb + 1) * 128],
                       ident8[:, c * 512:(c + 1) * 512], start=False, stop=True)
                return z, zres

            nxt = qk(0)
            for kb in range(t + 1):
                z, zres = nxt
                if kb + 1 <= t:
                    nxt = qk(kb + 1)
                pt = PT[ptrot[0] % NPT]
                ptrot[0] += 1
                op("act", "activation", zres, [pt], pt[:, :], z[:, :], AF.Exp, scale=0.125)
                for c in range(2):
                    op("pe", "matmul", [va_sb, pt], [oacc_res[c]], oacc[0:65, c * 512:(c + 1) * 512], va3[:, kb, :], pt[:, c * 512:(c + 1) * 512],
                       start=(kb == 0), stop=(kb == t))
                if kb == 1:
                    flush()

            def store():
                S_.dma("pool", E.oaT_d[ti].rearrange("d h q -> d (h q)"), oaT_s[sl][:, :], [oaT_s[sl]], [E.oaT_d], oaT_s[sl])
            normalize(oacc[0:65, :], oacc_res, 1024, oaT_s[sl][:, :], oaT_s[sl], after=store)

        def mla_heads(g, heads, ntg):
            qh = {}
            for h in heads:
                qt = qbT_h[qhrot[0] % 3]
                qhrot[0] += 1
                S_.dma("sp", qt[0:96, :], E.qbT_d[b, :, h, 512 * g:512 * g + 512], [E.qbT_d], [qt], qt)
                qh[h] = qt
            nkb = min(4 * g + 4, nts)
            units = [(h, kb) for h in heads for kb in range(nkb)]

            def qk(h, kb):
                zb = pbank[zrot[0] % 4]
                zrot[0] += 1
                diag = kb >= 4 * g
                op("pe", "matmul", [kbT_sb, qh[h]], [zb], zb[:, 0:512], kbT3[:, h, kb * 128:(kb + 1) * 128], qh[h][:, :],
                   start=True, stop=(not diag))
                if diag:
                    op("pe", "matmul", [ident_bf, maskT[kb - 4 * g]], [zb], zb[:, 0:512], ident_bf[:, :], maskT[kb - 4 * g][:, :],
                       start=False, stop=True)
                return zb

            nxt = qk(*units[0])
            for i, (h, kb) in enumerate(units):
                zb = nxt
                if i + 1 < len(units):
                    nxt = qk(*units[i + 1])
                oacc = pbank[4 + (h % 2)]
                pt = PT[ptrot[0] % NPT]
                ptrot[0] += 1
                op("act", "activation", [zb], [pt], pt[:, 0:512], zb[:, 0:512], AF.Exp, scale=96.0 ** -0.5)
                op("pe", "matmul", [vb_sb, pt], [oacc], oacc[0:65, 0:512], vb4[:, kb, h, :], pt[:, 0:512], start=(kb == 0), stop=(kb == nkb - 1))
                if kb == min(2, nkb - 1):
                    flush()
                if kb == nkb - 1:
                    ot = obT_h[ohrot[0] % 2]
                    ohrot[0] += 1

                    def store(ot=ot, h=h):
                        ti0 = b * NTS + 4 * g
                        S_.dma("pool", E.obT_d[ti0:ti0 + ntg, :, h, :].rearrange("t d q -> d t q"),
                               ot[:, 0:ntg * 128].rearrange("p (t q) -> p t q", q=128), [ot], [E.obT_d], ot)
                    normalize(oacc[0:65, 0:512], [oacc], 512, ot[:, :], ot, after=store)

        qa_load(0)
        dsa_index(0)
        dsa_bisect(0)
        if nts > 1:
            dsa_index(1)
        for t in range(nts):
            g, j = divmod(t, 4)
            ntg = min(4, nts - 4 * g)
            if t + 1 < nts:
                qa_load(t + 1)
            if t + 2 < nts:
                dsa_index(t + 2)
            if t + 1 < nts:
                dsa_bisect(t + 1)
            hs = [2 * j, 2 * j + 1]
            if j == ntg - 1:
                hs = list(range(2 * j, 8))
            mla_heads(g, hs, ntg)
            dsa_attn(t)
        flush()
    S_.pop()


def phase3(S_, envd):
    E = Env(envd)
    op = S_.op
    sb, pbank, P2 = E.sb, E.pbank, E.P2
    ident_f, ident_bf, ones_f = E.ident_f, E.ident_bf, E.ones_f
    rstd_from_ss, rms_heads, bcast_load, junk_act = E.rstd_from_ss, E.rms_heads, E.bcast_load, E.junk_act
    ntl = E.ntl
    S_.push()

    def pbf(i):
        return pbank[i][:, :].bitcast(BF16)

    wbrA = sb("wbrA", [128, 4 * D], BF16)
    wbrB = sb("wbrB", [128, 4 * D], BF16)
    wout = sb("wout", [128, 8 * D], BF16)
    wq_m = sb("wq_m", [128, 8 * 256], BF16)
    wkv_m = sb("wkv_m", [128, 8 * 512], BF16)
    wo_m = sb("wo_m", [64, 4 * D], BF16)
    wbrA3 = wbrA[:, :].rearrange("p (h n) -> p h n", h=4)
    wbrB3 = wbrB[:, :].rearrange("p (h n) -> p h n", h=4)
    wout3 = wout[:, :].rearrange("p (k n) -> p k n", k=8)
    wq3 = wq_m[:, :].rearrange("p (k n) -> p k n", k=8)
    wkv3 = wkv_m[:, :].rearrange("p (k n) -> p k n", k=8)
    wo3 = wo_m[:, :].rearrange("p (h n) -> p h n", h=4)
    for h in range(8):
        if h < 4:
            S_.dma("pool", wbrA3[:, h, :], E.w_bra_d[h * 128:(h + 1) * 128, :], [E.w_bra_d], [wbrA], wbrA)
            S_.dma("pool", wbrB3[:, h, :], E.w_brb_d[h * 128:(h + 1) * 128, :], [E.w_brb_d], [wbrB], wbrB)
        S_.dma("pool", wout3[:, h, :], E.w_out_d[h * 128:(h + 1) * 128, :], [E.w_out_d], [wout], wout)
        S_.dma("pool", wq3[:, h, :], E.mem_wq_d[h * 128:(h + 1) * 128, :], [E.mem_wq_d], [wq_m], wq_m)
        S_.dma("pool", wkv3[:, h, :], E.mem_wkv_d[h * 128:(h + 1) * 128, :], [E.mem_wkv_d], [wkv_m], wkv_m)
    for h in range(4):
        S_.dma("pool", wo3[:, h, :], E.mem_wo_d[h * 64:(h + 1) * 64, :], [E.mem_wo_d], [wo_m], wo_m)
    g_memx = bcast_load("g_memx", E.memx_g_d, D)
    g_mem = bcast_load("g_mem", E.mem_g_d, D)
    g_mq = bcast_load("g_memq", E.memq_g_d, 64)
    g_mk = bcast_load("g_memk", E.memk_g_d, 64)

    xin = [sb("x3in%d" % i, [128, D]) for i in range(2)]
    oaT = [sb("oaT_l%d" % i, [128, 512], BF16) for i in range(2)]
    obT = [sb("obT_l%d" % i, [128, 512], BF16) for i in range(2)]
    sg = [sb("sg_l%d" % i, [128, 2048], BF16) for i in range(2)]
    mg_f = sb("mg_f", [128, D])
    mg_t = sb("mg_t", [128, D])
    mg_bf = sb("mg_bf", [128, D], BF16)
    mT = sb("mT", [128, D], BF16)
    x1b = [sb("x1_%d" % i, [128, D]) for i in range(2)]
    ss = sb("ss3", [128, 16])
    r1 = sb("r13", [128, 16])
    rstd = sb("rstd3", [128, 16])
    h2 = sb("h2", [128, D], BF16)
    h2T = sb("h2T", [128, D], BF16)
    qm_f = sb("qm_f", [128, 256])
    qm_n = sb("qm_n", [128, 256])
    qm_sb = sb("qm_sb", [128, 256], BF16)
    qmT = sb("qmT", [64, 512], BF16)
    t_sq = sb("t3_sq", [128, 256])
    t_xn = sb("t3_xn", [128, 256])
    t_ssq = sb("t3_ssq", [128, 16])
    t_r1 = sb("t3_r1", [128, 16])
    t_rstd = sb("t3_rstd", [128, 16])
    tmps = (t_sq, t_ssq, t_r1, t_rstd, t_xn)
    kmT = sb("kmT", [64, 4 * 256], BF16)
    vm = sb("vm", [128, 2 * 4 * 65], BF16)
    kmT3 = kmT[:, :].rearrange("p (h m) -> p h m", h=4)
    vm4 = vm[:, :].rearrange("p (b h c) -> p b h c", b=2, h=4)
    memt = sb("memt", [128, D])
    mn = sb("mn", [128, D], BF16)
    mnT = sb("mnT", [128, D], BF16)
    kvm_f = sb("kvm_f", [128, 512])
    km_n = sb("km_n", [128, 256])
    km_sb = sb("km_sb", [128, 256], BF16)
    PTm = [sb("PTm%d" % i, [128, 512], BF16) for i in range(2)]
    oa_sb = sb("oa3_sb", [65, 512])
    r3_bf = sb("r3_bf", [65, 512], BF16)
    oaU3 = sb("oaU3", [64, 512], BF16)
    ones3_bf = sb("ones3_bf", [65, 64], BF16)
    op("dve", "memset", [], [ones3_bf], ones3_bf[:, :], 1.0)
    omT = sb("omT", [64, 512], BF16)
    x2s = [sb("x2s%d" % i, [128, D]) for i in range(2)]
    op("dve", "memset", [], [vm], vm[:, :], 1.0)
    zr = [0]

    def zbank():
        z = pbank[zr[0] % 4]
        zr[0] += 1
        return z

    zrA = [0]
    zrB = [0]

    def zbankA():
        z = pbank[zrA[0] % 2]
        zrA[0] += 1
        return z

    def zbankB():
        z = pbank[2 + zrB[0] % 2]
        zrB[0] += 1
        return z

    def evac_copy(dst_ap, dst_res, src_ap, src_res, flip=[0]):
        if flip[0] % 2 == 0:
            op("act", "copy", [src_res], [dst_res], dst_ap, src_ap)
        else:
            op("dve", "tensor_copy", [src_res], [dst_res], dst_ap, src_ap)
        flip[0] += 1

    def norm_to_bf(src, gbc, dst):
        op("act", "activation", [src], [ss], junk_act[:, :], src[:, :], AF.Square, accum_out=ss[:, 0:1])
        rstd_from_ss(ss, rstd, 1, D, r1)
        op("dve", "scalar_tensor_tensor", [src, rstd, gbc], [dst], dst[:, :], src[:, :], rstd[:, 0:1], gbc[:, :], ALU.mult, ALU.mult)

    def transpose8(src_bf, dstT, bank):
        for kc in range(8):
            op("pe", "transpose", [src_bf, ident_bf], [pbank[bank]], pbf(bank)[:, kc * 128:(kc + 1) * 128], src_bf[:, kc * 128:(kc + 1) * 128], ident_bf[:, :])
        op("act", "copy", [pbank[bank]], [dstT], dstT[:, :], pbf(bank)[:, :])

    ntile_seq = [min(NTS, max(0, ntl - b * NTS)) for b in range(NB)]
    for b in range(NB):
        if ntile_seq[b] == 0:
            continue
        for mb in range(2):
            S_.dma("sp", memt[:, :], E.mem_d[b * 256 + mb * 128:b * 256 + (mb + 1) * 128, :], [E.mem_d], [memt], memt)
            norm_to_bf(memt, g_mem, mn)
            transpose8(mn, mnT, 4)
            zb = zbank()
            for kc in range(8):
                op("pe", "matmul", [mnT, wkv_m], [zb], zb[:, 0:512], mnT[:, kc * 128:(kc + 1) * 128], wkv3[:, kc, :], start=(kc == 0), stop=(kc == 7))
            op("act", "copy", [zb], [kvm_f], kvm_f[:, :], zb[:, 0:512])
            rms_heads(kvm_f[:, 0:256].rearrange("p (h d) -> p h d", h=4), kvm_f, km_n[:, :].rearrange("p (h d) -> p h d", h=4), km_n, 4, 64, g_mk, tmps)
            op("dve", "tensor_copy", [km_n], [km_sb], km_sb[:, :], km_n[:, :])
            for h in range(4):
                op("pe", "transpose", [km_sb, ident_bf], [pbank[5]], pbf(5)[0:64, h * 128:(h + 1) * 128], km_sb[:, h * 64:(h + 1) * 64], ident_bf[:, :])
            op("act", "copy", [pbank[5]], [kmT], kmT3[:, :, mb * 128:(mb + 1) * 128], pbf(5)[0:64, 0:512].rearrange("p (h m) -> p h m", h=4))
            op("dve", "tensor_copy", [kvm_f], [vm], vm4[:, mb, :, 0:64], kvm_f[:, 256:512].rearrange("p (h d) -> p h d", h=4))
        def stA(t):
            ti = b * NTS + t
            sl = ti % 2
            xt = xin[sl]
            x1 = x1b[ti % 2]
            S_.dma("sp", xt[:, :], E.x_d[ti * 128:(ti + 1) * 128, :], [E.x_d], [xt], xt)
            for hh in range(2):
                S_.dma("sp", oaT[sl][hh * 64:(hh + 1) * 64, :].rearrange("p (j q) -> p j q", j=4),
                       E.oaT_d[ti].rearrange("d (j hh) q -> hh d j q", hh=2)[hh], [E.oaT_d], [oaT[sl]], oaT[sl])
                S_.dma("sp", obT[sl][hh * 64:(hh + 1) * 64, :].rearrange("p (j q) -> p j q", j=4),
                       E.obT_d[ti].rearrange("d (j hh) q -> hh d j q", hh=2)[hh], [E.obT_d], [obT[sl]], obT[sl])
            S_.dma("sp", sg[sl][:, :], E.sg_d[ti], [E.sg_d], [sg[sl]], sg[sl])
            for (src, w3, goff, first) in ((oaT[sl], wbrA3, 0, True), (obT[sl], wbrB3, 1024, False)):
                wres = wbrA if first else wbrB
                for nchunk in range(2):
                    zb = zbankA()
                    for h in range(4):
                        op("pe", "matmul", [src, wres], [zb], zb[:, 0:512], src[:, h * 128:(h + 1) * 128], w3[:, h, nchunk * 512:(nchunk + 1) * 512],
                           start=(h == 0), stop=(h == 3))
                    dst = mg_f if first else mg_t
                    op("dve", "tensor_tensor", [zb, sg[sl]], [dst], dst[:, nchunk * 512:(nchunk + 1) * 512], zb[:, 0:512],
                       sg[sl][:, goff + nchunk * 512:goff + (nchunk + 1) * 512], ALU.mult)
                    yield
            op("pool", "tensor_tensor", [mg_f, mg_t], [mg_bf], mg_bf[:, :], mg_f[:, :], mg_t[:, :], ALU.add)
            yield
            transpose8(mg_bf, mT, 4)
            yield
            for nchunk in range(2):
                zb = zbankA()
                for kc in range(8):
                    op("pe", "matmul", [mT, wout], [zb], zb[:, 0:512], mT[:, kc * 128:(kc + 1) * 128], wout3[:, kc, nchunk * 512:(nchunk + 1) * 512],
                       start=(kc == 0), stop=(kc == 7))
                op("dve", "tensor_tensor", [zb, xt], [x1], x1[:, nchunk * 512:(nchunk + 1) * 512], zb[:, 0:512], xt[:, nchunk * 512:(nchunk + 1) * 512], ALU.add)
                yield

        def stB(t):
            ti = b * NTS + t
            sl = ti % 2
            x1 = x1b[ti % 2]
            norm_to_bf(x1, g_memx, h2)
            yield
            transpose8(h2, h2T, 5)
            yield
            zb = zbankB()
            for kc in range(8):
                op("pe", "matmul", [h2T, wq_m], [zb], zb[:, 0:256], h2T[:, kc * 128:(kc + 1) * 128], wq3[:, kc, :], start=(kc == 0), stop=(kc == 7))
            op("act", "copy", [zb], [qm_f], qm_f[:, :], zb[:, 0:256])
            yield
            rms_heads(qm_f[:, :].rearrange("p (h d) -> p h d", h=4), qm_f, qm_n[:, :].rearrange("p (h d) -> p h d", h=4), qm_n, 4, 64, g_mq, tmps)
            op("dve", "tensor_copy", [qm_n], [qm_sb], qm_sb[:, :], qm_n[:, :])
            yield
            for h in range(4):
                op("pe", "transpose", [qm_sb, ident_bf], [pbank[5]], pbf(5)[0:64, h * 128:(h + 1) * 128], qm_sb[:, h * 64:(h + 1) * 64], ident_bf[:, :])
            op("act", "copy", [pbank[5]], [qmT], qmT[:, :], pbf(5)[0:64, 0:512])
            yield
            for mb in range(2):
                zb = zbankB()
                for h in range(4):
                    op("pe", "matmul", [kmT, qmT], [zb], zb[:, h * 128:(h + 1) * 128], kmT3[:, h, mb * 128:(mb + 1) * 128], qmT[:, h * 128:(h + 1) * 128],
                       start=True, stop=True)
                op("act", "activation", [zb], [PTm[mb]], PTm[mb][:, :], zb[:, 0:512], AF.Exp, scale=0.125)
            yield
            oacc = pbank[6]
            for h in range(4):
                for mb in range(2):
                    op("pe", "matmul", [vm, PTm[mb]], [oacc], oacc[0:65, h * 128:(h + 1) * 128], vm4[:, mb, h, :], PTm[mb][:, h * 128:(h + 1) * 128],
                       start=(mb == 0), stop=(mb == 1))
            op("act", "activation", [oacc], [oa_sb], oa_sb[64:65, :], oacc[64:65, 0:512], AF.Ln)
            op("act", "activation", [oa_sb], [r3_bf], r3_bf[64:65, :], oa_sb[64:65, :], AF.Exp, scale=-1.0)
            op("act", "copy", [oacc], [oaU3], oaU3[:, :], oacc[0:64, 0:512])
            yield
            op("pe", "matmul", [ones3_bf, r3_bf], [pbank[7]], pbank[7][0:64, 0:512], ones3_bf[64:65, 0:64], r3_bf[64:65, :], start=True, stop=True)
            op("dve", "tensor_tensor", [oaU3, pbank[7]], [omT], omT[:, :], oaU3[:, :], pbank[7][0:64, 0:512], ALU.mult)
            yield
            x2t = x2s[sl]
            for nchunk in range(2):
                zb = zbankB()
                for h in range(4):
                    op("pe", "matmul", [omT, wo_m], [zb], zb[:, 0:512], omT[:, h * 128:(h + 1) * 128], wo3[:, h, nchunk * 512:(nchunk + 1) * 512],
                       start=(h == 0), stop=(h == 3))
                op("dve", "tensor_tensor", [zb, x1], [x2t], x2t[:, nchunk * 512:(nchunk + 1) * 512], zb[:, 0:512], x1[:, nchunk * 512:(nchunk + 1) * 512], ALU.add)
            S_.dma("pool", E.x2_d[ti * 128:(ti + 1) * 128, :], x2t[:, :], [x2t], [E.x2_d], x2t)

        nts_ = ntile_seq[b]

        def interleave(gens):
            alive = list(gens)
            while alive:
                for g_ in list(alive):
                    try:
                        next(g_)
                    except StopIteration:
                        alive.remove(g_)

        interleave([stA(0)])
        for t in range(nts_):
            gs_ = [stB(t)]
            if t + 1 < nts_:
                gs_.insert(0, stA(t + 1))
            interleave(gs_)
    S_.pop()


def phase4(S_, envd):
    E = Env(envd)
    op = S_.op
    sb, pbank, P2 = E.sb, E.pbank, E.P2
    ident_f, ident_bf = E.ident_f, E.ident_bf
    rstd_from_ss, bcast_load, junk_act = E.rstd_from_ss, E.bcast_load, E.junk_act
    ntl = E.ntl
    S_.push()

    def pbf(i):
        return pbank[i][:, :].bitcast(BF16)

    wd = sb("wd", [128, 16 * 2 * D], BF16)
    wd4 = wd[:, :].rearrange("p (e f n) -> p e f n", e=16, f=2)
    for e in range(16):
        S_.dma("pool", wd4[:, e, :, :], E.moe_down_d[e].rearrange("(f p) n -> p f n", p=128), [E.moe_down_d], [wd], wd)
    wr = sb("wr", [128, 8 * 20], BF16)
    wr3 = wr[:, :].rearrange("p (k n) -> p k n", k=8)
    S_.dma("pool", wr3[:, :, 0:4], E.moe_wg_d[:, :].rearrange("(k p) n -> p k n", p=128), [E.moe_wg_d], [wr], wr)
    S_.dma("pool", wr3[:, :, 4:20], E.moe_we_d[:, :].rearrange("(k p) n -> p k n", p=128), [E.moe_we_d], [wr], wr)
    g_moe = bcast_load("g_moe", E.moe_g_d, D)
    bias_bc = bcast_load("bias_bc", E.moe_b_d, 16)
    sel = sb("sel_e", [16, 16 * 128], BF16)
    op("dve", "tensor_copy", [ident_f], [sel], sel[:, :].rearrange("p (e m) -> p e m", e=16),
       ident_f[0:16, 0:16].unsqueeze(2).broadcast_to([16, 16, 128]))

    x2g2 = [sb("x2g%d" % i, [128, 4 * D]) for i in range(2)]
    h3 = sb("h3", [128, D], BF16)
    h3T2 = [sb("h3T%d" % i, [128, 8 * 512], BF16) for i in range(2)]
    ss = sb("ss4", [128, 16])
    r1 = sb("r14", [128, 16])
    rstd = sb("rstd4", [128, 16])
    lg = sb("lg", [128, 20])
    sm = {n: sb("r_" + n, [128, 16]) for n in ("nlmax", "e4", "esum", "pg", "oh", "aff", "bi", "m1", "k1", "bi2", "m2", "k2", "sel", "asel", "den", "fac", "comb")}
    cT2 = [sb("cT%d" % i, [16, 512]) for i in range(2)]
    cH2 = [sb("cH%d" % i, [16, 512], BF16) for i in range(2)]
    cL2 = [sb("cL%d" % i, [16, 512], BF16) for i in range(2)]
    comb4 = [sb("comb4_%d" % i, [128, 16]) for i in range(4)]
    cbc = sb("cbc", [128, 512])
    sgl = [sb("sgl%d" % i, [128, 512], BF16) for i in range(2)]
    tu = [sb("tu%d" % i, [128, 512], BF16) for i in range(2)]
    actc = sb("actc", [128, 16 * 2 * 512], BF16)
    actc4 = actc[:, :].rearrange("p (e f q) -> p e f q", e=16, f=2)
    wgu = [sb("wgu%d" % i, [128, 8 * 512], BF16) for i in range(3)]
    o_s = [sb("o_s%d" % i, [128, D]) for i in range(2)]
    zr = [0]

    def zbank():
        z = pbank[zr[0] % 4]
        zr[0] += 1
        return z

    ngr = (ntl + 3) // 4
    wrot = [0]
    orot = [0]
    def pre_(g):
        ntg = min(4, ntl - 4 * g)
        W = ntg * 128
        x2g = x2g2[g % 2]
        h3T = h3T2[g % 2]
        h3T3 = h3T[:, :].rearrange("p (k q) -> p k q", k=8)
        cT = cT2[g % 2]
        cH = cH2[g % 2]
        cL = cL2[g % 2]
        for j in range(ntg):
            ti = 4 * g + j
            xs_ = x2g[:, j * D:(j + 1) * D]
            S_.dma("sp", xs_, E.x2_d[ti * 128:(ti + 1) * 128, :], [E.x2_d], [x2g], x2g)
        yield
        for j in range(ntg):
            xs_ = x2g[:, j * D:(j + 1) * D]
            op("act", "activation", [x2g], [ss], junk_act[:, :], xs_, AF.Square, accum_out=ss[:, 0:1])
            rstd_from_ss(ss, rstd, 1, D, r1)
            op("dve", "scalar_tensor_tensor", [x2g, rstd, g_moe], [h3], h3[:, :], xs_, rstd[:, 0:1], g_moe[:, :], ALU.mult, ALU.mult)
            yield
            for kc in range(8):
                op("pe", "transpose", [h3, ident_bf], [pbank[4]], pbf(4)[:, kc * 128:(kc + 1) * 128], h3[:, kc * 128:(kc + 1) * 128], ident_bf[:, :])
            op("act", "copy", [pbank[4]], [h3T], h3T3[:, :, j * 128:(j + 1) * 128], pbf(4)[:, :].rearrange("p (k q) -> p k q", k=8))
            rb = pbank[5]
            for kc in range(8):
                op("pe", "matmul", [h3T, wr], [rb], rb[:, 0:20], h3T3[:, kc, j * 128:(j + 1) * 128], wr3[:, kc, :], start=(kc == 0), stop=(kc == 7))
            op("dve", "tensor_copy", [rb], [lg], lg[:, :], rb[:, 0:20])
            yield
            m = sm
            op("dve", "tensor_reduce", [lg], [m["nlmax"]], m["nlmax"][:, 0:1], lg[:, 0:4], AX.X, ALU.max, negate=True)
            op("act", "activation", [lg, m["nlmax"]], [m["e4"], m["esum"]], m["e4"][:, 0:4], lg[:, 0:4], AF.Exp, bias=m["nlmax"][:, 0:1],
               accum_out=m["esum"][:, 0:1])
            op("dve", "reciprocal", [m["esum"]], [m["pg"]], m["pg"][:, 0:1], m["esum"][:, 0:1])
            op("dve", "tensor_scalar", [lg, m["nlmax"]], [m["oh"]], m["oh"][:, 0:4], lg[:, 0:4], m["nlmax"][:, 0:1], 0.0, ALU.add, ALU.is_ge)
            op("act", "activation", [lg], [m["aff"]], m["aff"][:, :], lg[:, 4:20], AF.Sigmoid)
            op("dve", "tensor_tensor", [m["aff"], bias_bc], [m["bi"]], m["bi"][:, :], m["aff"][:, :], bias_bc[:, :], ALU.add)
            v3 = lambda n: m[n][:, :].rearrange("p (g e) -> p g e", g=4)
            b4 = lambda n: m[n][:, 0:4].unsqueeze(2).broadcast_to([128, 4, 4])
            op("dve", "tensor_reduce", [m["bi"]], [m["m1"]], m["m1"][:, 0:4], v3("bi"), AX.X, ALU.max)
            op("dve", "tensor_tensor", [m["bi"], m["m1"]], [m["k1"]], v3("k1"), v3("bi"), b4("m1"), ALU.is_ge)
            op("dve", "scalar_tensor_tensor", [m["k1"], m["bi"]], [m["bi2"]], m["bi2"][:, :], m["k1"][:, :], -1e9, m["bi"][:, :], ALU.mult, ALU.add)
            op("dve", "tensor_reduce", [m["bi2"]], [m["m2"]], m["m2"][:, 0:4], v3("bi2"), AX.X, ALU.max)
            op("dve", "tensor_tensor", [m["bi2"], m["m2"]], [m["k2"]], v3("k2"), v3("bi2"), b4("m2"), ALU.is_ge)
            op("dve", "tensor_tensor", [m["k1"], m["k2"]], [m["sel"]], m["sel"][:, :], m["k1"][:, :], m["k2"][:, :], ALU.add)
            op("dve", "tensor_tensor", [m["sel"], m["oh"]], [m["sel"]], v3("sel"), v3("sel"), b4("oh"), ALU.mult)
            op("dve", "tensor_tensor", [m["aff"], m["sel"]], [m["asel"]], m["asel"][:, :], m["aff"][:, :], m["sel"][:, :], ALU.mult)
            op("dve", "tensor_reduce", [m["asel"]], [m["den"]], m["den"][:, 0:1], m["asel"][:, :], AX.X, ALU.add)
            op("dve", "reciprocal", [m["den"]], [m["den"]], m["den"][:, 0:1], m["den"][:, 0:1])
            op("dve", "tensor_tensor", [m["den"], m["pg"]], [m["fac"]], m["fac"][:, 0:1], m["den"][:, 0:1], m["pg"][:, 0:1], ALU.mult)
            op("dve", "tensor_scalar", [m["asel"], m["fac"]], [m["comb"]], m["comb"][:, :], m["asel"][:, :], m["fac"][:, 0:1], None, ALU.mult)
            op("pool", "tensor_copy", [m["comb"]], [comb4[j]], comb4[j][:, :], m["comb"][:, :])
            yield
        for j in range(ntg):
            op("pe", "transpose", [comb4[j], ident_f], [pbank[6]], pbank[6][0:16, j * 128:(j + 1) * 128], comb4[j][:, :], ident_f[:, :])
        op("dve", "tensor_copy", [pbank[6]], [cT], cT[:, 0:W], pbank[6][0:16, 0:W])
        op("dve", "tensor_copy", [cT], [cH], cH[:, 0:W], cT[:, 0:W])
        op("dve", "tensor_tensor", [cT, cH], [cL], cL[:, 0:W], cT[:, 0:W], cH[:, 0:W], ALU.subtract)

    def experts_(g, gen=None):
        ntg = min(4, ntl - 4 * g)
        W = ntg * 128
        x2g = x2g2[g % 2]
        h3T = h3T2[g % 2]
        h3T3 = h3T[:, :].rearrange("p (k q) -> p k q", k=8)
        cT = cT2[g % 2]
        cH = cH2[g % 2]
        cL = cL2[g % 2]
        for e in range(16):
            wt = wgu[wrot[0] % 3]
            wrot[0] += 1
            wt3 = wt[:, :].rearrange("p (k f) -> p k f", k=8)
            S_.dma("sp", wt3, E.wgu_d[e], [E.wgu_d], [wt], wt)
            cb = pbank[7]
            op("pe", "matmul", [sel, cH], [cb], cb[:, 0:W], sel[:, e * 128:(e + 1) * 128], cH[:, 0:W], start=True, stop=False)
            op("pe", "matmul", [sel, cL], [cb], cb[:, 0:W], sel[:, e * 128:(e + 1) * 128], cL[:, 0:W], start=False, stop=True)
            op("act", "copy", [cb], [cbc], cbc[:, 0:W], cb[:, 0:W])
            for fc in range(2):
                pg_ = zbank()
                for kc in range(8):
                    op("pe", "matmul", [wt, h3T], [pg_], pg_[:, 0:W], wt3[:, kc, fc * 128:(fc + 1) * 128], h3T3[:, kc, 0:W], start=(kc == 0), stop=(kc == 7))
                pu_ = zbank()
                for kc in range(8):
                    op("pe", "matmul", [wt, h3T], [pu_], pu_[:, 0:W], wt3[:, kc, 256 + fc * 128:256 + (fc + 1) * 128], h3T3[:, kc, 0:W],
                       start=(kc == 0), stop=(kc == 7))
                op("act", "activation", [pg_], [sgl[fc]], sgl[fc][:, 0:W], pg_[:, 0:W], AF.Silu)
                op("dve", "tensor_tensor", [pu_, cbc], [tu[fc]], tu[fc][:, 0:W], pu_[:, 0:W], cbc[:, 0:W], ALU.mult)
                op("pool", "tensor_tensor", [sgl[fc], tu[fc]], [actc], actc4[:, e, fc, 0:W], sgl[fc][:, 0:W], tu[fc][:, 0:W], ALU.mult)
            if gen is not None:
                next(gen, None)

    def down_(g):
        ntg = min(4, ntl - 4 * g)
        W = ntg * 128
        x2g = x2g2[g % 2]
        h3T = h3T2[g % 2]
        h3T3 = h3T[:, :].rearrange("p (k q) -> p k q", k=8)
        cT = cT2[g % 2]
        cH = cH2[g % 2]
        cL = cL2[g % 2]
        for j in range(ntg):
            ti = 4 * g + j
            ot = o_s[orot[0] % 2]
            orot[0] += 1
            for nchunk in range(2):
                zb = pbank[4 + nchunk]
                for e in range(16):
                    for fc in range(2):
                        op("pe", "matmul", [actc, wd], [zb], zb[:, 0:512], actc4[:, e, fc, j * 128:(j + 1) * 128], wd4[:, e, fc, nchunk * 512:(nchunk + 1) * 512],
                           start=(e == 0 and fc == 0), stop=(e == 15 and fc == 1))
                op("dve", "tensor_tensor", [zb, x2g], [ot], ot[:, nchunk * 512:(nchunk + 1) * 512], zb[:, 0:512],
                   x2g[:, j * D + nchunk * 512:j * D + (nchunk + 1) * 512], ALU.add)
            S_.dma("pool", E.out_d[ti * 128:(ti + 1) * 128, :], ot[:, :], [ot], [E.out_d], ot)

    for _ in pre_(0):
        pass
    for g in range(ngr):
        gen = pre_(g + 1) if g + 1 < ngr else None
        experts_(g, gen)
        if gen is not None:
            for _ in gen:
                pass
        down_(g)
    S_.pop()


_INPUT_ORDER = None


def kernel(**inputs):
    nc, stack = build()
    in_maps = make_in_maps(inputs)
    res = run_bass_kernel_spmd(nc, in_maps, core_ids=list(range(NCORES)))
    out = np.concatenate([np.asarray(r["out"]).reshape(NB, S, D) for r in res.results], axis=0)
    return out.astype(np.float32)


def make_in_maps(inputs):
    x = np.ascontiguousarray(np.asarray(inputs["x"], dtype=np.float32))
    mem = np.ascontiguousarray(np.asarray(inputs["mem"], dtype=np.float32))
    pos = np.ascontiguousarray(np.asarray(inputs["positions"], dtype=np.int32))
    maps = []
    for c in range(NCORES):
        m = {}
        m["x"] = x[c * NB:(c + 1) * NB].reshape(NB * S, D)
        m["mem"] = mem[c * NB:(c + 1) * NB].reshape(NB * 256, D)
        m["positions"] = pos[c * NB:(c + 1) * NB].reshape(NT, 128)
        for k, v in inputs.items():
            if k in ("x", "mem", "positions"):
                continue
            a = np.ascontiguousarray(np.asarray(v, dtype=np.float32))
            a = a[0]
            if a.ndim == 1:
                a = a.reshape(1, -1)
            m[k] = a
        maps.append(m)
    return maps
```
